# Optimizing a Trainium2 kernel written in Bass

```python
import math
import jax
import jax.numpy as jnp
from jax import lax
import numpy as np


D_MODEL = 1024
BATCH = 8
SEQ = 4096
DEPTH = 1

D_MIX = D_MODEL
D_LRU = D_MIX // 2
LRU_BLOCKS = 8
LRU_BLOCK = D_LRU // LRU_BLOCKS
LRU_C = 8.0
CONV_W = 4
CONV_LEFT = 2
N_ATT_HEADS = 4
D_ATT = D_MIX - D_LRU
HEAD_DV = D_ATT // N_ATT_HEADS
HEAD_DK = HEAD_DV // 2
QK_W = N_ATT_HEADS * 2 * HEAD_DK
D_IN_PROJ = 2 * D_LRU + 2 * QK_W + D_ATT
SPLITS = [D_LRU, 2 * D_LRU, 2 * D_LRU + QK_W, 2 * D_LRU + 2 * QK_W]
N_BUCKETS = 32
MAX_DISTANCE = 128
Q_BLOCK = 128
N_EXPERTS = 16
EC_FACTOR = 2
D_FF_EXPERT = D_MODEL
EPS = 1e-6

kernel_name = 'hybrid_rglru_diffattn_ecmoe_block'


def rms_norm(x, g):
    xf = x.astype(jnp.float32)
    y = xf * lax.rsqrt(jnp.mean(xf * xf, axis=-1, keepdims=True) + EPS)
    return (y * g.astype(jnp.float32)).astype(x.dtype)


def t5_buckets(rel):
    half = N_BUCKETS // 2
    max_exact = half // 2
    ret = jnp.where(rel > 0, half, 0)
    n = jnp.abs(rel)
    nf = jnp.maximum(n, 1).astype(jnp.float32)
    large = max_exact + (jnp.log(nf / max_exact) / math.log(MAX_DISTANCE / max_exact)
                         * (half - max_exact)).astype(jnp.int32)
    large = jnp.minimum(large, half - 1)
    return ret + jnp.where(n < max_exact, n, large)


def rg_lru(xc, w_a, b_a, w_x, b_x, lam, reverse):
    B, S, _ = xc.shape
    xb = xc.reshape(B, S, LRU_BLOCKS, LRU_BLOCK)
    r = jax.nn.sigmoid((jnp.einsum('bsnc,ncd->bsnd', xb, w_a).reshape(B, S, D_LRU) + b_a).astype(jnp.float32))
    i = jax.nn.sigmoid((jnp.einsum('bsnc,ncd->bsnd', xb, w_x).reshape(B, S, D_LRU) + b_x).astype(jnp.float32))
    log_a = LRU_C * r * jax.nn.log_sigmoid(lam.astype(jnp.float32))
    a = jnp.exp(log_a)
    u = jnp.sqrt(-jnp.expm1(2.0 * log_a)) * (i * xc.astype(jnp.float32))

    def combine(left, right):
        a1, b1 = left
        a2, b2 = right
        return a1 * a2, a2 * b1 + b2

    _, h = lax.associative_scan(combine, (a, u), axis=1, reverse=reverse)
    return h.astype(xc.dtype)


def diff_attention(q, k, v, g_q, g_k, lam, g_o, bias_table, lam_init):
    B, S = q.shape[0], q.shape[1]
    q = rms_norm(q, g_q) * (HEAD_DK ** -0.5)
    k = rms_norm(k, g_k)
    nb = S // Q_BLOCK
    qb = q.reshape(B, nb, Q_BLOCK, N_ATT_HEADS, 2, HEAD_DK).transpose(1, 0, 2, 3, 4, 5)
    kpos = jnp.arange(S, dtype=jnp.int32)

    def block(args):
        qi, start = args
        qpos = start + jnp.arange(Q_BLOCK, dtype=jnp.int32)
        bias = bias_table[t5_buckets(kpos[None, :] - qpos[:, None])].astype(jnp.float32)
        bias = bias.transpose(2, 0, 1)
        s = jnp.einsum('bqhjd,bkhjd->bhjqk', qi, k).astype(jnp.float32) + bias[None, :, None]
        p = jax.nn.softmax(s, axis=-1)
        w = (p[:, :, 0] - lam * p[:, :, 1]).astype(v.dtype)
        return jnp.einsum('bhqk,bkhd->bqhd', w, v)

    o = lax.map(block, (qb, jnp.arange(nb, dtype=jnp.int32) * Q_BLOCK))
    o = o.transpose(1, 0, 2, 3, 4).reshape(B, S, N_ATT_HEADS, HEAD_DV)
    o = rms_norm(o, g_o) * (1.0 - lam_init)
    return o.reshape(B, S, D_ATT)


def expert_choice_ffn(h, w_router, w1, w3, w2):
    B, S, D = h.shape
    cap = max(1, EC_FACTOR * S // N_EXPERTS)
    aff = jax.nn.softmax(jnp.dot(h, w_router).astype(jnp.float32), axis=-1)
    g, idx = lax.top_k(aff.transpose(0, 2, 1), cap)
    xe = jax.vmap(lambda hb, ib: hb[ib])(h, idx)
    a = jnp.einsum('becd,edf->becf', xe, w1)
    b = jnp.einsum('becd,edf->becf', xe, w3)
    y = jnp.einsum('becf,efd->becd', jax.nn.silu(a) * b, w2)
    y = y * g[..., None].astype(y.dtype)
    return jax.vmap(lambda ib, yb: jnp.zeros((S, D), yb.dtype).at[ib.reshape(-1)].add(yb.reshape(-1, D)))(idx, y)


def setup_inputs(seed: int = 0) -> dict:
    key = jax.random.key(seed)
    ks = jax.random.split(key, 26)
    f32 = jnp.float32

    def nrm(k, shape, fan_in):
        return jax.random.normal(k, shape, f32) * (fan_in ** -0.5)

    def gain(k, shape):
        return 1.0 + 0.02 * jax.random.normal(k, shape, f32)

    a0 = jax.random.uniform(ks[12], (DEPTH, 2, D_LRU), f32, minval=0.9, maxval=0.999)
    return {
        'x': jax.random.normal(ks[0], (BATCH, SEQ, D_MODEL), f32),
        'c': jax.random.normal(ks[1], (BATCH, D_MODEL), f32),
        'w_mod': nrm(ks[2], (DEPTH, D_MODEL, 6 * D_MODEL), D_MODEL),
        'b_mod': 0.02 * jax.random.normal(ks[3], (DEPTH, 6 * D_MODEL), f32),
        'g_norm1': gain(ks[4], (DEPTH, D_MODEL)),
        'w_in': nrm(ks[5], (DEPTH, D_MODEL, D_IN_PROJ), D_MODEL),
        'conv_w': nrm(ks[6], (DEPTH, CONV_W, 1, D_LRU), CONV_W),
        'conv_b': 0.02 * jax.random.normal(ks[7], (DEPTH, D_LRU), f32),
        'lru_w_a': nrm(ks[8], (DEPTH, 2, LRU_BLOCKS, LRU_BLOCK, LRU_BLOCK), LRU_BLOCK),
        'lru_b_a': 0.1 * jax.random.normal(ks[9], (DEPTH, 2, D_LRU), f32),
        'lru_w_x': nrm(ks[10], (DEPTH, 2, LRU_BLOCKS, LRU_BLOCK, LRU_BLOCK), LRU_BLOCK),
        'lru_b_x': 0.1 * jax.random.normal(ks[11], (DEPTH, 2, D_LRU), f32),
        'lru_lambda': jnp.log(a0) - jnp.log1p(-a0),
        'g_q': gain(ks[13], (DEPTH, HEAD_DK)),
        'g_k': gain(ks[14], (DEPTH, HEAD_DK)),
        'lambda_qk': 0.1 * jax.random.normal(ks[15], (DEPTH, 4, HEAD_DK), f32),
        'g_attn_out': gain(ks[16], (DEPTH, HEAD_DV)),
        'rel_bias': 0.5 * jax.random.normal(ks[17], (N_BUCKETS, N_ATT_HEADS), f32),
        'w_out': nrm(ks[18], (DEPTH, D_MIX, D_MODEL), D_MIX),
        'g_norm2': gain(ks[19], (DEPTH, D_MODEL)),
        'w_router': nrm(ks[20], (DEPTH, D_MODEL, N_EXPERTS), D_MODEL),
        'w1': nrm(ks[21], (DEPTH, N_EXPERTS, D_MODEL, D_FF_EXPERT), D_MODEL),
        'w3': nrm(ks[22], (DEPTH, N_EXPERTS, D_MODEL, D_FF_EXPERT), D_MODEL),
        'w2': nrm(ks[23], (DEPTH, N_EXPERTS, D_FF_EXPERT, D_MODEL), D_FF_EXPERT),
    }


def reference(x, c, w_mod, b_mod, g_norm1, w_in, conv_w, conv_b, lru_w_a, lru_b_a, lru_w_x, lru_b_x,
              lru_lambda, g_q, g_k, lambda_qk, g_attn_out, rel_bias, w_out, g_norm2, w_router, w1, w3, w2):
    B, S, _ = x.shape
    for l in range(DEPTH):
        mod = jnp.dot(jax.nn.silu(c), w_mod[l]) + b_mod[l]
        shift1, scale1, gate1, shift2, scale2, gate2 = jnp.split(mod[:, None, :], 6, axis=-1)

        h = rms_norm(x, g_norm1[l]) * (1.0 + scale1) + shift1
        proj = jnp.dot(h, w_in[l])
        x_lru, z_lru, q, k, v = jnp.split(proj, SPLITS, axis=-1)

        xc = lax.conv_general_dilated(x_lru, conv_w[l], (1,), [(CONV_LEFT, CONV_W - 1 - CONV_LEFT)],
                                      dimension_numbers=('NWC', 'WIO', 'NWC'),
                                      feature_group_count=D_LRU) + conv_b[l]
        h_fwd = rg_lru(xc, lru_w_a[l, 0], lru_b_a[l, 0], lru_w_x[l, 0], lru_b_x[l, 0], lru_lambda[l, 0], False)
        h_bwd = rg_lru(xc, lru_w_a[l, 1], lru_b_a[l, 1], lru_w_x[l, 1], lru_b_x[l, 1], lru_lambda[l, 1], True)
        y_lru = (h_fwd + h_bwd) * jax.nn.gelu(z_lru)

        lam_init = 0.8 - 0.6 * math.exp(-0.3 * l)
        lq = lambda_qk[l].astype(jnp.float32)
        lam = jnp.exp(jnp.sum(lq[0] * lq[1])) - jnp.exp(jnp.sum(lq[2] * lq[3])) + lam_init
        y_att = diff_attention(q.reshape(B, S, N_ATT_HEADS, 2, HEAD_DK),
                               k.reshape(B, S, N_ATT_HEADS, 2, HEAD_DK),
                               v.reshape(B, S, N_ATT_HEADS, HEAD_DV),
                               g_q[l], g_k[l], lam, g_attn_out[l], rel_bias, lam_init)

        mix = jnp.dot(jnp.concatenate([y_lru, y_att], axis=-1), w_out[l])
        x = x + gate1 * mix

        h2 = rms_norm(x, g_norm2[l]) * (1.0 + scale2) + shift2
        x = x + gate2 * expert_choice_ffn(h2, w_router[l], w1[l], w3[l], w2[l])
    return x
```

```python
import math
from contextlib import ExitStack

import numpy as np
import ml_dtypes

import concourse.bass as bass
import concourse.mybir as mybir
from concourse.bass_utils import run_bass_kernel_spmd

F32 = mybir.dt.float32
BF16 = mybir.dt.bfloat16
I32 = mybir.dt.int32
AF = mybir.ActivationFunctionType
ALU = mybir.AluOpType
AX = mybir.AxisListType

S = 4096
D = 1024
NT = 32
NG = 8
NE = 16
CAP = 512
EPS = 1e-6
LAM_INIT = 0.2
C1 = math.sqrt(2.0 / math.pi)

SM = {}
_off = 0
for _n, _w in [("c", 8), ("bmod", 48), ("g1", 8), ("g2", 8), ("convw", 16), ("convb", 4), ("ba", 8), ("bx", 8),
               ("lam", 8), ("gq", 1), ("gk", 1), ("lqk", 256), ("rb", 128), ("go", 128), ("wr", 128)]:
    SM[_n] = (_off, _off + _w)
    _off += _w
NS = _off


def _bucket_table():
    out = np.zeros(511, np.int64)
    for i in range(511):
        rel = i - 255
        ret = 16 if rel > 0 else 0
        n = abs(rel)
        nf = np.float32(max(n, 1))
        large = 8 + int(np.float32(np.float32(np.log(np.float32(nf / np.float32(8.0)))) / np.float32(math.log(16.0)))
                        * np.float32(8.0))
        large = min(large, 15)
        out[i] = ret + (n if n < 8 else large)
    return out


def host_consts():
    ident = np.eye(128, dtype=np.float32)
    b64 = np.zeros((128, 128), np.float32)
    b64[:64, :64] = 1
    b64[64:, 64:] = 1
    ut = np.triu(np.ones((128, 128), np.float32))
    bt = _bucket_table()
    ohrev = np.zeros((32, 511), np.float32)
    for i in range(511):
        ohrev[bt[(255 - i) + 255], i] = 1.0
    p = np.arange(128)[:, None, None]
    t = np.arange(32)[None, :, None]
    tok = np.broadcast_to(t * 128 + p, (128, 32, 16))
    tokab = np.stack([tok // 64, tok % 64], axis=1).astype(np.float32)
    iota1 = np.broadcast_to(np.arange(1, 513, dtype=np.float32)[None, :], (128, 512))
    pp = np.arange(128)
    gsum = (pp[:, None] % 16 == pp[None, :] % 16).astype(np.float32)
    return {
        "gsum": gsum,
        "tokab": np.ascontiguousarray(tokab).astype(ml_dtypes.bfloat16),
        "iota1": np.ascontiguousarray(iota1),
        "ident_f": ident,
        "ident_b": ident.astype(ml_dtypes.bfloat16),
        "ones_f": np.ones((128, 128), np.float32),
        "b64_b": b64.astype(ml_dtypes.bfloat16),
        "ut_b": ut.astype(ml_dtypes.bfloat16),
        "ones_b": np.ones((128, 128), ml_dtypes.bfloat16),
        "ohrev": ohrev,
    }


def pack_smalls(inp, b):
    sm = np.zeros((128, NS), np.float32)

    def colmaj(v, nchunk):
        return np.ascontiguousarray(np.asarray(v, np.float32).reshape(nchunk, 128).T)

    def put(name, arr):
        a, e = SM[name]
        sm[:, a:e] = arr

    put("c", colmaj(inp["c"][b], 8))
    put("bmod", colmaj(inp["b_mod"][0], 48))
    put("g1", colmaj(inp["g_norm1"][0], 8))
    put("g2", colmaj(inp["g_norm2"][0], 8))
    cw = np.asarray(inp["conv_w"][0], np.float32).reshape(4, 512)
    put("convw", np.ascontiguousarray(cw.reshape(4, 4, 128).transpose(2, 1, 0)).reshape(128, 16))
    put("convb", colmaj(inp["conv_b"][0], 4))
    for nm, key in (("ba", "lru_b_a"), ("bx", "lru_b_x"), ("lam", "lru_lambda")):
        v = np.asarray(inp[key][0], np.float32)
        put(nm, np.concatenate([colmaj(v[0], 4), colmaj(v[1], 4)], axis=1))
    put("gq", np.tile(np.asarray(inp["g_q"][0], np.float32), 2).reshape(128, 1))
    put("gk", np.tile(np.asarray(inp["g_k"][0], np.float32), 2).reshape(128, 1))
    put("lqk", np.tile(np.asarray(inp["lambda_qk"][0], np.float32).reshape(1, 256), (128, 1)))
    put("rb", np.tile(np.asarray(inp["rel_bias"], np.float32).reshape(1, 128), (128, 1)))
    put("go", np.tile(np.asarray(inp["g_attn_out"][0], np.float32).reshape(1, 128), (128, 1)))
    wr = np.asarray(inp["w_router"][0], np.float32)
    put("wr", np.ascontiguousarray(wr.reshape(8, 128, 16).transpose(1, 0, 2)).reshape(128, 128))
    return sm


class TK:
    ENG = ("pe", "act", "dve", "pool", "sp")

    def __init__(self, nc, ctx):
        self.nc = nc
        self.ctx = ctx
        self.eng = {"pe": nc.tensor, "act": nc.scalar, "dve": nc.vector, "pool": nc.gpsimd, "sp": nc.sync}
        self.semh = {}
        self.cnt = {}
        for e in self.ENG:
            self.semh[e] = ctx.enter_context(nc.semaphore("s_" + e))
            self.cnt[e] = 0
        self.waited = {e: {} for e in self.ENG}
        self.last_w = {}
        self.readers = {}

    def _wait(self, e, stamp):
        if stamp is None:
            return
        sk, val = stamp
        if sk == "pe" and e == "pe":
            return
        if sk == e:
            assert val <= self.cnt[e], ("self-wait on future inc", e, val, self.cnt[e])
        if self.waited[e].get(sk, 0) >= val:
            return
        self.eng[e].wait_ge(self.semh[sk], val)
        self.waited[e][sk] = val

    def _deps(self, e, r, w):
        need = {}

        def add(stamp):
            if stamp is not None:
                need[stamp[0]] = max(need.get(stamp[0], 0), stamp[1])

        for k in r:
            add(self.last_w.get(k))
        for k in w:
            add(self.last_w.get(k))
            for sk, val in self.readers.get(k, {}).items():
                add((sk, val))
        for sk, val in need.items():
            self._wait(e, (sk, val))

    def _record(self, stamp, r, w):
        sk, val = stamp
        for k in r:
            d = self.readers.setdefault(k, {})
            d[sk] = max(d.get(sk, 0), val)
        for k in w:
            self.last_w[k] = stamp
            self.readers[k] = {}

    def op(self, e, fn, r=(), w=(), inc=True):
        self._deps(e, r, w)
        ins = fn(self.eng[e])
        if inc:
            self.cnt[e] += 1
            ins.then_inc(self.semh[e], 1)
            stamp = (e, self.cnt[e])
        else:
            stamp = (e, self.cnt[e] + 1)
        self._record(stamp, r, w)
        return ins

    def dma(self, q, out, in_, r=(), w=(), stream="d", indirect=None, **kw):
        self._deps(q, r, w)
        sk = "d:" + stream
        if sk not in self.semh:
            self.semh[sk] = self.ctx.enter_context(self.nc.semaphore("s_" + stream))
            self.cnt[sk] = 0
        if indirect is None:
            ins = self.eng[q].dma_start(out=out, in_=in_, **kw)
        else:
            ins = self.eng[q].indirect_dma_start(out=out, in_=in_, **indirect, **kw)
        self.cnt[sk] += 16
        ins.then_inc(self.semh[sk], 16)
        self._record((sk, self.cnt[sk]), r, w)
        return ins

    def barrier(self):
        for e in self.ENG:
            for sk, c in self.cnt.items():
                if c > 0 and not (sk == "pe" and e == "pe"):
                    self._wait(e, (sk, c))
        self.last_w = {}
        self.readers = {}

    def wait_all(self, e):
        for sk, c in self.cnt.items():
            if sk != e and c > 0:
                self._wait(e, (sk, c))


class Bld:
    def __init__(self, debug=None):
        self.debug = debug
        self.nc = bass.Bass("TRN2", target_bir_lowering=False)
        self.root = ExitStack()
        self.tk = TK(self.nc, self.root)

    def dram_in(self, name, shape, dt):
        return self.nc.dram_tensor(name, list(shape), dt, kind="ExternalInput").ap()

    def dram_scratch(self, name, shape, dt):
        kind = "ExternalOutput" if (self.debug and name in self.debug) else "Internal"
        return self.nc.dram_tensor(name, list(shape), dt, kind=kind).ap()


def sb(ctx, nc, name, shape, dt):
    return ctx.enter_context(nc.sbuf_tensor(name, list(shape), dt))


def ps(ctx, nc, name, shape, dt):
    return ctx.enter_context(nc.psum_tensor(name, list(shape), dt))


def build(debug=None, stop_after=None, opts=()):
    B = Bld(debug)
    nc, tk = B.nc, B.tk
    root = B.root
    x_d = B.dram_in("x", [S, D], F32)
    sm_d = B.dram_in("smalls", [128, NS], F32)
    wmod_d = B.dram_in("w_mod", [D, 6 * D], F32)
    win_d = B.dram_in("w_in", [D, 2560], F32)
    lwa_d = B.dram_in("lru_w_a", [2, 8, 64, 64], F32)
    lwx_d = B.dram_in("lru_w_x", [2, 8, 64, 64], F32)
    wout_d = B.dram_in("w_out", [D, D], F32)
    w1_d = B.dram_in("w1", [NE, D, D], F32)
    w3_d = B.dram_in("w3", [NE, D, D], F32)
    w2_d = B.dram_in("w2", [NE, D, D], F32)
    rbk_d = B.dram_in("rel_bias", [32, 4, 128], F32)
    cst = {}
    for nm, shp, dt in [("ident_f", [128, 128], F32), ("ident_b", [128, 128], BF16), ("ones_f", [128, 128], F32),
                        ("b64_b", [128, 128], BF16), ("ut_b", [128, 128], BF16), ("ones_b", [128, 128], BF16),
                        ("ohrev", [32, 511], F32), ("tokab", [128, 2, 32, 16], BF16), ("iota1", [128, 512], F32),
                        ("gsum", [128, 128], F32)]:
        cst[nm] = B.dram_in(nm, shp, dt)
    out_d = nc.dram_tensor("out", [S, D], F32, kind="ExternalOutput").ap()
    xl_d = B.dram_scratch("xl_d", [4, 128, S], F32)
    gz_d = B.dram_scratch("gz_d", [4, 128, S], BF16)
    qT_d = B.dram_scratch("qT_d", [4, 128, S], BF16)
    kT_d = B.dram_scratch("kT_d", [4, 128, S], BF16)
    v_d = B.dram_scratch("v_d", [S, 512], BF16)
    yT_d = B.dram_scratch("yT_d", [8, 128, S], BF16)
    f_d = B.dram_scratch("f_d", [4, 128, 511], F32)
    h2_d = B.dram_scratch("h2_d", [S, D], BF16)

    smalls = sb(root, nc, "smalls_sb", [128, NS], F32)
    ident_f = sb(root, nc, "ident_f_sb", [128, 128], F32)
    ident_b = sb(root, nc, "ident_b_sb", [128, 128], BF16)
    ones_f = sb(root, nc, "ones_f_sb", [128, 128], F32)
    b64_b = sb(root, nc, "b64_b_sb", [128, 128], BF16)
    modT = sb(root, nc, "modT", [128, 48], F32)
    A1 = sb(root, nc, "A1", [128, 8], F32)
    A2 = sb(root, nc, "A2", [128, 8], F32)
    g1bc = sb(root, nc, "g1bc", [128, D], F32)
    g2bc = sb(root, nc, "g2bc", [128, D], F32)
    A2bc = sb(root, nc, "A2bc", [128, D], F32)
    B2bc = sb(root, nc, "B2bc", [128, D], F32)
    affT = sb(root, nc, "affT", [128, NT, NE], F32)
    epsc = sb(root, nc, "epsc", [128, 1], F32)
    mhalf = sb(root, nc, "mhalf", [128, 8], F32)

    def smc(name, i=None, j=None):
        a, e = SM[name]
        if i is None:
            return smalls[:, a:e]
        return smalls[:, a + i:a + (j if j is not None else i + 1)]

    tk.dma("sp", smalls[:], sm_d, w=["smalls"], stream="c0")
    tk.dma("sp", ident_f[:], cst["ident_f"], w=["ident_f"], stream="c1")
    tk.dma("sp", ident_b[:], cst["ident_b"], w=["ident_b"], stream="c2")
    tk.dma("sp", ones_f[:], cst["ones_f"], w=["ones_f"], stream="c3")
    tk.dma("sp", b64_b[:], cst["b64_b"], w=["b64_b"], stream="c4")
    tk.op("pool", lambda g: g.memset(mhalf[:], -0.5), w=["mhalf"])
    tk.op("pool", lambda g: g.memset(epsc[:], EPS), w=["epsc"])

    with ExitStack() as ph:
        win_b = sb(ph, nc, "win_b", [128, 8, 2560], BF16)
        xt = [sb(ph, nc, f"xt{i}", [128, D], F32) for i in range(4)]
        junk = sb(ph, nc, "junk", [128, D], BF16)
        xn = [sb(ph, nc, f"xn{i}", [128, D], BF16) for i in range(4)]
        ss = sb(ph, nc, "ss", [128, NT], F32)
        ms = sb(ph, nc, "ms", [128, NT], F32)
        rstd = sb(ph, nc, "rstd", [128, NT], F32)
        hT = [sb(ph, nc, f"hT{i}", [128, 8, 512], BF16) for i in range(2)]
        tp = [ps(ph, nc, f"tp{i}", [128, 8, 128], BF16) for i in range(2)]
        acc = [ps(ph, nc, f"acc{i}", [128, 512], F32) for i in range(4)]
        ssb = [ps(ph, nc, f"ssb{i}", [128, 512], F32) for i in range(2)]
        st_f = [sb(ph, nc, f"st_f{i}", [128, 512], F32) for i in range(2)]
        st_b = [sb(ph, nc, f"st_b{i}", [128, 512], BF16) for i in range(4)]
        z2 = [sb(ph, nc, f"z2{i}", [128, 512], F32) for i in range(2)]
        zu = [sb(ph, nc, f"zu{i}", [128, 512], F32) for i in range(2)]
        zt = [sb(ph, nc, f"zt{i}", [128, 512], F32) for i in range(2)]
        sq = [sb(ph, nc, f"sq{i}", [128, 512], BF16) for i in range(2)]
        msq = [sb(ph, nc, f"msq{i}", [128, 512], F32) for i in range(2)]
        rsq = [sb(ph, nc, f"rsq{i}", [128, 512], F32) for i in range(2)]
        gq8 = sb(ph, nc, "gq8", [128, 1], F32)
        sc_t = sb(ph, nc, "sc_t", [128, 8], F32)
        sc_f = sb(ph, nc, "sc_f", [128, 8], F32)
        sc_b = sb(ph, nc, "sc_b", [128, 8], BF16)
        wm = [sb(ph, nc, f"wm{i}", [128, 8, 1024], BF16) for i in range(2)]
        diag = [sb(ph, nc, f"diag{i}", [128, 128], F32) for i in range(2)]
        tk.op("dve", lambda v: v.tensor_scalar(out=gq8[:], in0=smc("gq"), scalar1=0.125, scalar2=None, op0=ALU.mult),
              r=["smalls"], w=["gq8"])
        ohrev = sb(ph, nc, "ohrev_sb", [32, 511], F32)
        rbk = sb(ph, nc, "rbk", [32, 4, 128], F32)
        fsb = sb(ph, nc, "fsb", [128, 4, 511], F32)
        tk.dma("sp", ohrev[:], cst["ohrev"], w=["ohrev"], stream="c5")
        tk.dma("sp", rbk[:], rbk_d, w=["rbk"], stream="c6")
        for h in range(4):
            fp = acc[h % 2][:, 0:511]
            tk.op("pe", lambda pe, h=h, fp=fp: pe.matmul(fp, lhsT=rbk[:, h, :], rhs=ohrev[:], start=True, stop=True),
                  r=["rbk", "ohrev"], w=[("acc", h % 2)])
            tk.op("dve", lambda v, h=h, fp=fp: v.tensor_copy(fsb[:, h, :], fp), r=[("acc", h % 2)], w=[("fsb", h)])
        tk.dma("sp", f_d.rearrange("h p n -> p h n"), fsb[:], r=[("fsb", h) for h in range(4)], w=["f_d"], stream="c7")
        wm_v = wmod_d.rearrange("(kc p) n -> p kc n", p=128)

        def load_wm(j):
            tk.dma("pool", wm[j % 2][:], wm_v[:, :, j * 1024:(j + 1) * 1024], w=[("wm", j % 2)], stream=f"wm{j % 2}")

        load_wm(0)
        load_wm(1)
        win_v = win_d.rearrange("(kc p) n -> p kc n", p=128)
        for kc in range(8):
            tk.dma("pool", win_b[:, kc, :], win_v[:, kc, :], w=[("win_b", kc)], stream=f"win{kc}")
        tk.op("act", lambda a: a.activation(out=sc_t[:], in_=smc("c"), func=AF.Tanh, scale=0.5),
              r=["smalls"], w=["sc_t"])
        tk.op("dve", lambda v: v.tensor_scalar(out=sc_f[:], in0=sc_t[:], scalar1=0.5, scalar2=0.5,
                                               op0=ALU.mult, op1=ALU.add), r=["sc_t"], w=["sc_f"])
        tk.op("dve", lambda v: v.tensor_tensor(out=sc_b[:], in0=sc_f[:], in1=smc("c"), op=ALU.mult),
              r=["sc_f", "smalls"], w=["sc_b"])

        def mod_mm(bank, j):
            for o8 in range(8):
                o = j * 8 + o8
                for kc in range(8):
                    tk.op("pe", lambda t, o=o, o8=o8, kc=kc: t.matmul(
                        bank[:, o:o + 1], lhsT=wm[j % 2][:, kc, o8 * 128:(o8 + 1) * 128], rhs=sc_b[:, kc:kc + 1],
                        start=(kc == 0), stop=(kc == 7)),
                        r=[("wm", j % 2), "sc_b"], w=[("acc", 3)] if bank is acc[3] else [], inc=(kc == 7 and o8 == 7))

        mod_mm(acc[3], 0)
        mod_mm(acc[3], 1)
        tk.op("dve", lambda v: v.tensor_tensor(out=modT[:, 0:16], in0=acc[3][:, 0:16], in1=smc("bmod", 0, 16),
                                               op=ALU.add), r=[("acc", 3), "smalls"], w=["modT"])
        tk.op("dve", lambda v: v.scalar_tensor_tensor(out=A1[:], in0=modT[:, 8:16], scalar=1.0, in1=smc("g1"),
                                                      op0=ALU.add, op1=ALU.mult), r=["modT", "smalls"], w=["A1"])
        load_wm(2)
        load_wm(3)

        def mk_mod(j0):
            st = {}

            def s0():
                a = cnt["acc"] % 4
                cnt["acc"] += 1
                st["a"] = a
                for j in (j0, j0 + 1):
                    for o8 in range(8):
                        o = j * 8 + o8
                        for kc in range(8):
                            tk.op("pe", lambda t, o=o, o8=o8, kc=kc, j=j: t.matmul(
                                acc[a][:, o:o + 1], lhsT=wm[j % 2][:, kc, o8 * 128:(o8 + 1) * 128],
                                rhs=sc_b[:, kc:kc + 1], start=(kc == 0), stop=(kc == 7)),
                                r=[("wm", j % 2), "sc_b"], w=[("acc", a)], inc=(kc == 7 and o8 == 7))
                if j0 + 3 < 6:
                    load_wm(j0 + 2)
                    load_wm(j0 + 3)

            def s1():
                a = st["a"]
                c0, c1 = j0 * 8, j0 * 8 + 16
                tk.op("dve", lambda v: v.tensor_tensor(out=modT[:, c0:c1], in0=acc[a][:, c0:c1],
                                                       in1=smc("bmod", c0, c1), op=ALU.add),
                      r=[("acc", a), "smalls"], w=["modT"])
                if j0 == 4:
                    tk.op("dve", lambda v: v.scalar_tensor_tensor(out=A2[:], in0=modT[:, 32:40], scalar=1.0,
                                                                  in1=smc("g2"), op0=ALU.add, op1=ALU.mult),
                          r=["modT", "smalls"], w=["A2"])

            return (s0, s1, lambda: None)

        def mk_bc(gi, half):
            srct, col0, dst = ((modT, 16, g1bc), (modT, 40, g2bc), (A2, 0, A2bc), (modT, 24, B2bc))[gi]
            st = {}

            def s0():
                a = cnt["acc"] % 4
                cnt["acc"] += 1
                st["a"] = a
                for k4 in range(4):
                    kc = half * 4 + k4
                    dslot = k4 % 2
                    tk.op("dve", lambda v, kc=kc, dslot=dslot: v.tensor_scalar(
                        out=diag[dslot][:], in0=ident_f[:], scalar1=srct[:, col0 + kc:col0 + kc + 1], scalar2=None,
                        op0=ALU.mult), r=["modT", "A2", "ident_f"], w=[("diag", dslot)])
                    tk.op("pe", lambda t, k4=k4, dslot=dslot: t.matmul(
                        acc[a][:, k4 * 128:(k4 + 1) * 128], lhsT=ones_f[:], rhs=diag[dslot][:], start=True, stop=True),
                        r=["ones_f", ("diag", dslot)], w=[("acc", a)])

            def s1():
                a = st["a"]
                tk.op("act", lambda e: e.copy(out=dst[:, half * 512:(half + 1) * 512], in_=acc[a][:]),
                      r=[("acc", a)], w=[("gbc", gi)])

            return (s0, s1, lambda: None)


        cnt = {"st_b": 0, "st_f": 0, "z": 0, "qk": 0, "acc": 0}

        def load_x(t):
            tk.dma("sp", xt[t % 4][:], x_d[t * 128:(t + 1) * 128, :], w=[("xt", t % 4)], stream=f"x{t % 4}")

        def front_a(t):
            s4, s2 = t % 4, t % 2
            if t + 2 < NT:
                load_x(t + 2)
            tk.op("act", lambda a: a.activation(out=junk[:], in_=xt[s4][:], func=AF.Square,
                                                accum_out=ss[:, t:t + 1]), r=[("xt", s4)], w=["junk", ("ss", t)])
            tk.op("act", lambda a: a.activation(out=ms[:, t:t + 1], in_=ss[:, t:t + 1], func=AF.Ln, scale=1.0 / D,
                                                bias=epsc[:, 0:1]), r=[("ss", t), "epsc"], w=[("ms", t)])
            tk.op("act", lambda a: a.activation(out=rstd[:, t:t + 1], in_=ms[:, t:t + 1], func=AF.Exp, scale=-0.5),
                  r=[("ms", t)], w=[("rstd", t)])
            tk.op("act", lambda a: a.activation(out=xn[s4][:], in_=xt[s4][:], func=AF.Copy,
                                                scale=rstd[:, t:t + 1]), r=[("xt", s4), ("rstd", t)], w=[("xn", s4)])

        def front_b(t):
            s2 = t % 2
            s4 = t % 4
            g, tl = t // 4, t % 4
            for kc in range(8):
                tk.op("pe", lambda pe, kc=kc: pe.transpose(tp[s2][:, kc, :], xn[s4][:, kc * 128:(kc + 1) * 128],
                                                           ident_b[:]),
                      r=[("xn", s4), "ident_b"], w=[("tp", s2)], inc=(kc == 7))
            for kc in range(8):
                tk.op("dve", lambda v, kc=kc: v.tensor_scalar(
                    out=hT[g % 2][:, kc, tl * 128:(tl + 1) * 128], in0=tp[s2][:, kc, :],
                    scalar1=A1[:, kc:kc + 1], scalar2=modT[:, kc:kc + 1], op0=ALU.mult, op1=ALU.add),
                    r=[("tp", s2), "A1", "modT"], w=[("hT", g % 2, tl)])

        def mk_fm(g, oc):
            st = {}
            tsl = slice(g * 512, (g + 1) * 512)
            hk = [("hT", g % 2, tl) for tl in range(4)]

            def s0():
                a = cnt["acc"] % 4
                cnt["acc"] += 1
                st["a"] = a
                for kc in range(8):
                    tk.op("pe", lambda pe, kc=kc: pe.matmul(acc[a][:], lhsT=win_b[:, kc, oc * 128:(oc + 1) * 128],
                                                            rhs=hT[g % 2][:, kc, :], start=(kc == 0), stop=(kc == 7)),
                          r=hk + [("win_b", kc)], w=[("acc", a)], inc=(kc == 7))
                if 4 <= oc < 8:
                    i = cnt["z"] % 2
                    cnt["z"] += 1
                    st["i"] = i
                    tk.op("act", lambda e: e.activation(out=z2[i][:], in_=acc[a][:], func=AF.Square),
                          r=[("acc", a)], w=[("z2", i)])
                    tk.op("dve", lambda v: v.tensor_scalar(out=z2[i][:], in0=z2[i][:], scalar1=C1 * 0.044715,
                                                           scalar2=C1, op0=ALU.mult, op1=ALU.add),
                          r=[("z2", i)], w=[("z2", i)])
                elif oc >= 8:
                    i = cnt["qk"] % 2
                    cnt["qk"] += 1
                    st["i"] = i
                    tk.op("act", lambda e: e.activation(out=sq[i][:], in_=acc[a][:], func=AF.Square),
                          r=[("acc", a)], w=[("sq", i)])

            def s1():
                a = st["a"]
                if oc < 4:
                    i = cnt["st_f"] % 2
                    cnt["st_f"] += 1
                    tk.op("dve", lambda v: v.tensor_copy(st_f[i][:], acc[a][:]), r=[("acc", a)], w=[("st_f", i)])
                    tk.dma("sp", xl_d[oc, :, tsl], st_f[i][:], r=[("st_f", i)], w=[("xl_d", oc, g)], stream=f"sf{i}")
                elif oc < 8:
                    i = st["i"]
                    tk.op("dve", lambda v: v.tensor_tensor(out=zu[i][:], in0=z2[i][:], in1=acc[a][:], op=ALU.mult),
                          r=[("z2", i), ("acc", a)], w=[("zu", i)])
                    tk.op("act", lambda e: e.activation(out=zt[i][:], in_=zu[i][:], func=AF.Tanh),
                          r=[("zu", i)], w=[("zt", i)])
                else:
                    i = st["i"]
                    tk.op("pe", lambda pe: pe.matmul(ssb[i][:], lhsT=b64_b[:], rhs=sq[i][:], start=True, stop=True),
                          r=["b64_b", ("sq", i)], w=[("ssb", i)])
                    tk.op("act", lambda e: e.activation(out=msq[i][:], in_=ssb[i][:], func=AF.Ln, scale=1.0 / 64,
                                                        bias=epsc[:, 0:1]), r=[("ssb", i), "epsc"], w=[("msq", i)])
                    tk.op("act", lambda e: e.activation(out=rsq[i][:], in_=msq[i][:], func=AF.Exp, scale=-0.5),
                          r=[("msq", i)], w=[("rsq", i)])

            def s2():
                a = st["a"]
                if oc < 4:
                    return
                j = cnt["st_b"] % 4
                cnt["st_b"] += 1
                i = st["i"]
                if oc < 8:
                    tk.op("dve", lambda v: v.scalar_tensor_tensor(out=st_b[j][:], in0=zt[i][:], scalar=1.0,
                                                                  in1=acc[a][:], op0=ALU.add, op1=ALU.mult),
                          r=[("zt", i), ("acc", a)], w=[("st_b", j)])
                    tk.dma("sp", gz_d[oc - 4, :, tsl], st_b[j][:], r=[("st_b", j)], w=[("gz_d", oc - 4, g)],
                           stream=f"sb{j}")
                else:
                    isq = oc < 12
                    gcol = gq8[:, 0:1] if isq else smc("gk")
                    tk.op("dve", lambda v: v.scalar_tensor_tensor(out=st_b[j][:], in0=acc[a][:], scalar=gcol,
                                                                  in1=rsq[i][:], op0=ALU.mult, op1=ALU.mult),
                          r=[("acc", a), ("rsq", i), "gq8", "smalls"], w=[("st_b", j)])
                    dst = qT_d if isq else kT_d
                    hd = (oc - 8) % 4
                    tk.dma("sp", dst[hd, :, tsl], st_b[j][:], r=[("st_b", j)], w=[("qk_d", oc, g)], stream=f"sb{j}")

            return (s0, s1, s2)

        def mk_v(g, tl):
            st = {}
            t = g * 4 + tl

            def s0():
                a = cnt["acc"] % 4
                cnt["acc"] += 1
                st["a"] = a
                for kc in range(8):
                    tk.op("pe", lambda pe, kc=kc: pe.matmul(acc[a][:], lhsT=hT[g % 2][:, kc, tl * 128:(tl + 1) * 128],
                                                            rhs=win_b[:, kc, 2048:2560], start=(kc == 0),
                                                            stop=(kc == 7)),
                          r=[("hT", g % 2, tl), ("win_b", kc)], w=[("acc", a)], inc=(kc == 7))

            def s1():
                a = st["a"]
                j = cnt["st_b"] % 4
                cnt["st_b"] += 1
                tk.op("dve", lambda v: v.tensor_copy(st_b[j][:], acc[a][:]), r=[("acc", a)], w=[("st_b", j)])
                tk.dma("sp", v_d[t * 128:(t + 1) * 128, :], st_b[j][:], r=[("st_b", j)], w=[("v_d", t)],
                       stream=f"sb{j}")

            return (s0, s1, lambda: None)

        load_x(0)
        load_x(1)
        front_a(0)
        front_a(1)
        front_b(0)
        front_a(2)
        front_b(1)
        front_a(3)
        front_b(2)
        front_b(3)
        chunks = []
        for g in range(NG):
            units = ([("fm", oc) for oc in range(4, 8)] + [("fm", oc) for oc in range(8, 16)]
                     + [("fm", oc) for oc in range(4)] + [("v", tl) for tl in range(4)])
            for ui, (kind, idx) in enumerate(units):
                chunks.append((mk_fm(g, idx) if kind == "fm" else mk_v(g, idx), g, ui))
            if g == 1:
                chunks.append((mk_mod(2), None, None))
            if g == 3:
                chunks.append((mk_mod(4), None, None))
            if g in (4, 5, 6, 7):
                gi = g - 4
                order = (2, 3, 0, 1)[gi] if False else gi
                chunks.append((mk_bc(order, 0), None, None))
                chunks.append((mk_bc(order, 1), None, None))
        for slot in range(len(chunks) + 2):
            if slot < len(chunks):
                chunks[slot][0][0]()
            if 0 <= slot - 1 < len(chunks):
                chunks[slot - 1][0][1]()
            if 0 <= slot - 2 < len(chunks):
                chunks[slot - 2][0][2]()
            if slot < len(chunks):
                _, g, ui = chunks[slot]
                if g is not None and g + 1 < NG:
                    if ui in (4, 6, 8, 10):
                        front_a((g + 1) * 4 + (ui - 4) // 2)
                    if ui in (8, 10, 12, 14):
                        front_b((g + 1) * 4 + (ui - 8) // 2)
        tk.barrier()
    if stop_after == "p1":
        return finish(B, out_d, [])

    with ExitStack() as ph:
        wbd = sb(ph, nc, "wbd", [128, 16, 128], BF16)
        lt = sb(ph, nc, "lt", [128, 8], F32)
        cl = sb(ph, nc, "cl", [128, 8], F32)
        hcl = sb(ph, nc, "hcl", [128, 8], F32)
        hba = sb(ph, nc, "hba", [128, 8], F32)
        hbx = sb(ph, nc, "hbx", [128, 8], F32)
        onec = sb(ph, nc, "onec", [128, 1], F32)
        xlp = sb(ph, nc, "xlp", [128, S + 4], F32)
        xc = [sb(ph, nc, f"xc{i}", [128, S], F32) for i in range(2)]
        xcb = sb(ph, nc, "xcb", [128, S], BF16)
        av = [sb(ph, nc, f"av{i}", [128, S], F32) for i in range(2)]
        a2v = [sb(ph, nc, f"a2v{i}", [128, S], F32) for i in range(2)]
        thxv = sb(ph, nc, "thxv", [128, S], F32)
        hf = sb(ph, nc, "hf", [128, S], F32)
        hb = sb(ph, nc, "hb", [128, S], F32)
        gzs = sb(ph, nc, "gzs", [128, S], BF16)
        th = [sb(ph, nc, f"th{i}", [128, 512], F32) for i in range(2)]
        pA = [ps(ph, nc, f"pA{i}", [128, 512], F32) for i in range(2)]
        pX = [ps(ph, nc, f"pX{i}", [128, 512], F32) for i in range(2)]
        tk.op("dve", lambda p: p.memset(wbd[:], 0.0), w=["wbd"])
        tk.op("dve", lambda p: p.memset(onec[:], 1.0), w=["onec"])
        tk.op("dve", lambda p: p.memset(xlp[:, 0:2], 0.0), w=["xlp_pad"])
        tk.op("dve", lambda p: p.memset(xlp[:, S + 2:S + 4], 0.0), w=["xlp_pad"])
        for gt, wsrc_ in enumerate((lwa_d, lwx_d)):
            for dr in range(2):
                for c in range(4):
                    for blk in range(2):
                        tk.dma("pool", wbd[blk * 64:(blk + 1) * 64, gt * 8 + dr * 4 + c, blk * 64:(blk + 1) * 64],
                               wsrc_[dr, 2 * c + blk], r=["wbd"], w=[("wbdl", gt, dr, c, blk)], stream="wbd")
        tk.op("act", lambda a: a.activation(out=lt[:], in_=smc("lam"), func=AF.Exp, scale=-1.0), r=["smalls"], w=["lt"])
        tk.op("act", lambda a: a.activation(out=lt[:], in_=lt[:], func=AF.Ln, bias=onec[:, 0:1]), r=["lt", "onec"],
              w=["lt"])
        tk.op("dve", lambda v: v.tensor_scalar(out=cl[:], in0=lt[:], scalar1=-8.0, scalar2=None, op0=ALU.mult),
              r=["lt"], w=["cl"])
        tk.op("dve", lambda v: v.tensor_scalar(out=hcl[:], in0=lt[:], scalar1=-4.0, scalar2=None, op0=ALU.mult),
              r=["lt"], w=["hcl"])
        tk.op("dve", lambda v: v.tensor_scalar(out=hba[:], in0=smc("ba"), scalar1=0.5, scalar2=None, op0=ALU.mult),
              r=["smalls"], w=["hba"])
        tk.op("dve", lambda v: v.tensor_scalar(out=hbx[:], in0=smc("bx"), scalar1=0.5, scalar2=None, op0=ALU.mult),
              r=["smalls"], w=["hbx"])
        wbd_keys = [("wbdl", gt, dr, c, blk) for gt in range(2) for dr in range(2) for c in range(4) for blk in range(2)]
        un = [0]

        def load_chunk(c):
            tk.dma("sp", xlp[:, 2:S + 2], xl_d[c], w=["xlp"], stream="xlp")

        def conv(c):
            xcc = xc[c % 2]
            cw = lambda j: smc("convw", c * 4 + j)
            tk.op("dve", lambda v: v.tensor_scalar(out=xcc[:], in0=xlp[:, 0:S], scalar1=cw(0), scalar2=smc("convb", c),
                                                   op0=ALU.mult, op1=ALU.add),
                  r=["xlp", "xlp_pad", "smalls"], w=[("xc", c % 2)])
            for j in range(1, 4):
                tk.op("dve", lambda v, j=j: v.scalar_tensor_tensor(out=xcc[:], in0=xlp[:, j:j + S], scalar=cw(j),
                                                                   in1=xcc[:], op0=ALU.mult, op1=ALU.add),
                      r=["xlp", "xlp_pad", "smalls", ("xc", c % 2)], w=[("xc", c % 2)])
            if c + 1 < 4:
                load_chunk(c + 1)

        def act_part(c, dr):
            col = dr * 4 + c
            for g in range(NG):
                i = un[0] % 2
                un[0] += 1
                ts_ = slice(g * 512, (g + 1) * 512)
                tk.op("pe", lambda pe: pe.matmul(pA[i][:], lhsT=wbd[:, 0 * 8 + col, :], rhs=xcb[:, ts_],
                                                 start=True, stop=True), r=["xcb", "wbd"] + wbd_keys, w=[("pA", i)])
                tk.op("act", lambda a: a.activation(out=th[i][:], in_=pA[i][:], func=AF.Tanh, scale=0.5,
                                                    bias=hba[:, col:col + 1]), r=[("pA", i), "hba"], w=[("th", i)])
                tk.op("act", lambda a: a.activation(out=av[dr][:, ts_], in_=th[i][:], func=AF.Exp,
                                                    scale=hcl[:, col:col + 1], bias=hcl[:, col:col + 1]),
                      r=[("th", i), "hcl"], w=[("av", dr)])
                tk.op("act", lambda a: a.activation(out=a2v[dr][:, ts_], in_=th[i][:], func=AF.Exp,
                                                    scale=cl[:, col:col + 1], bias=cl[:, col:col + 1]),
                      r=[("th", i), "cl"], w=[("a2v", dr)])
            for g in range(NG):
                i = un[0] % 2
                un[0] += 1
                ts_ = slice(g * 512, (g + 1) * 512)
                tk.op("pe", lambda pe: pe.matmul(pX[i][:], lhsT=wbd[:, 1 * 8 + col, :], rhs=xcb[:, ts_],
                                                 start=True, stop=True), r=["xcb", "wbd"] + wbd_keys, w=[("pX", i)])
                tk.op("act", lambda a: a.activation(out=thxv[:, ts_], in_=pX[i][:], func=AF.Tanh, scale=0.5,
                                                    bias=hbx[:, col:col + 1]), r=[("pX", i), "hbx"], w=["thxv"])
            tk.op("act", lambda a: a.activation(out=a2v[dr][:], in_=a2v[dr][:], func=AF.Ln, scale=-1.0,
                                                bias=onec[:, 0:1]), r=[("a2v", dr), "onec"], w=[("a2v", dr)])
            tk.op("act", lambda a: a.activation(out=a2v[dr][:], in_=a2v[dr][:], func=AF.Exp, scale=0.5),
                  r=[("a2v", dr)], w=[("a2v", dr)])

        def dve_part(c, dr):
            xcc = xc[c % 2]
            tk.op("dve", lambda v: v.scalar_tensor_tensor(out=thxv[:], in0=thxv[:], scalar=1.0, in1=xcc[:],
                                                          op0=ALU.add, op1=ALU.mult),
                  r=["thxv", ("xc", c % 2)], w=["thxv"])
            tk.op("dve", lambda v: v.scalar_tensor_tensor(out=a2v[dr][:], in0=thxv[:], scalar=0.5, in1=a2v[dr][:],
                                                          op0=ALU.mult, op1=ALU.mult),
                  r=["thxv", ("a2v", dr)], w=[("a2v", dr)])
            if dr == 0:
                tk.op("dve", lambda v: v.tensor_tensor_scan(out=hf[:], data0=av[0][:], data1=a2v[0][:], initial=0.0,
                                                            op0=ALU.mult, op1=ALU.add),
                      r=[("av", 0), ("a2v", 0)], w=["hf"])
            else:
                tk.op("dve", lambda v: v.tensor_tensor_scan(out=hb[:, ::-1], data0=av[1][:, ::-1],
                                                            data1=a2v[1][:, ::-1], initial=0.0, op0=ALU.mult,
                                                            op1=ALU.add), r=[("av", 1), ("a2v", 1)], w=["hb"])

        def finish_chunk(c):
            tk.op("dve", lambda v: v.tensor_tensor(out=hf[:], in0=hf[:], in1=hb[:], op=ALU.add),
                  r=["hf", "hb"], w=["hf"])
            tk.op("dve", lambda v: v.scalar_tensor_tensor(out=gzs[:], in0=hf[:], scalar=0.5, in1=gzs[:],
                                                          op0=ALU.mult, op1=ALU.mult), r=["hf", "gzs"], w=["gzs"])
            tk.dma("sp", yT_d[c], gzs[:], r=["gzs"], w=[("yT_d", c)], stream="ys")

        load_chunk(0)
        conv(0)
        for c in range(4):
            tk.dma("sp", gzs[:], gz_d[c], w=["gzs"], stream="gzs")
            tk.op("act", lambda a: a.copy(out=xcb[:], in_=xc[c % 2][:]), r=[("xc", c % 2)], w=["xcb"])
            act_part(c, 0)
            dve_part(c, 0)
            act_part(c, 1)
            if c + 1 < 4:
                conv(c + 1)
            dve_part(c, 1)
            finish_chunk(c)
        tk.barrier()
    if stop_after == "p2":
        return finish(B, out_d, [])

    with ExitStack() as ph:
        Tt = sb(ph, nc, "Tt", [128, 12, 128], F32)
        Dh = sb(ph, nc, "Dh", [128, 24, 512], BF16)
        Dl = sb(ph, nc, "Dl", [128, 24, 512], BF16)
        ncf = sb(ph, nc, "ncf", [128, 8], F32)
        lpr = sb(ph, nc, "lpr", [128, 128], F32)
        lsum = sb(ph, nc, "lsum", [128, 2], F32)
        lexp = sb(ph, nc, "lexp", [128, 2], F32)
        nlam = sb(ph, nc, "nlam", [128, 1], F32)
        gos = sb(ph, nc, "gos", [128, 128], F32)
        junk3 = sb(ph, nc, "junk3", [128, 128], F32)
        qh = [sb(ph, nc, f"qh{i}", [128, S], BF16) for i in range(2)]
        kh = [sb(ph, nc, f"kh{i}", [128, S], BF16) for i in range(2)]
        vh = [sb(ph, nc, f"vh{i}", [128, NT, 129], BF16) for i in range(2)]
        PT = [sb(ph, nc, f"PT{i}", [128, 1024], BF16) for i in range(3)]
        ocp = sb(ph, nc, "ocp", [128, 3, 3, 160], F32)
        rz = sb(ph, nc, "rz", [128, 9], F32)
        o0 = [sb(ph, nc, f"o0{i}", [128, 128], F32) for i in range(2)]
        oo4 = sb(ph, nc, "oo4", [128, 4, 128], F32)
        yb4 = sb(ph, nc, "yb4", [128, 4, 128], BF16)
        oss = sb(ph, nc, "oss", [128, 4], F32)
        oms = sb(ph, nc, "oms", [128, 4], F32)
        orstd = sb(ph, nc, "orstd", [128, 4], F32)
        yb = [sb(ph, nc, f"yb{i}", [128, 128], BF16) for i in range(2)]
        yst = [sb(ph, nc, f"yst{i}", [128, 512], BF16) for i in range(2)]
        sps = [ps(ph, nc, f"sps{i}", [128, 1024], F32) for i in range(2)]
        oacc = [ps(ph, nc, f"oacc{i}", [128, 512], F32) for i in range(3)]
        tpo = ps(ph, nc, "tpo", [128, 8, 128], BF16)

        def load_head(h):
            i = h % 2
            tk.dma("sp", qh[i][:], qT_d[h], w=[("qh", i)], stream=f"qh{i}")
            tk.dma("sp", kh[i][:], kT_d[h], w=[("kh", i)], stream=f"kh{i}")
            tk.dma("sp", vh[i][:, :, 0:128], v_d[:, h * 128:(h + 1) * 128].rearrange("(t p) d -> p t d", p=128),
                   r=[("vh1", i)], w=[("vh", i)], stream=f"vh{i}")

        for i in range(2):
            tk.op("dve", lambda p, i=i: p.memset(vh[i][:, :, 128:129], 1.0), w=[("vh1", i)])
        tk.op("dve", lambda p: p.memset(ocp[:], 1.0), w=[("ocp", 0), ("ocp", 1), ("ocp", 2)])
        load_head(0)
        for h in range(4):
            for dl in range(3):
                src = bass.AP(tensor=f_d.tensor, offset=h * 128 * 511 + 255 - 128 * (dl - 1), ap=[[510, 128], [1, 128]])
                tk.dma("sp", Tt[:, h * 3 + dl, :], src, w=[("Tt", h, dl)], stream=f"tt{h * 3 + dl}")
        tk.op("dve", lambda v: v.tensor_scalar(out=ncf[:, 0:4], in0=smc("rb", 60, 64), scalar1=-1.0, scalar2=None,
                                               op0=ALU.mult), r=["smalls"], w=["ncf"])
        tk.op("dve", lambda v: v.tensor_scalar(out=ncf[:, 4:8], in0=smc("rb", 124, 128), scalar1=-1.0, scalar2=None,
                                               op0=ALU.mult), r=["smalls", "ncf"], w=["ncf"])
        for h in range(4):
            for dk in range(-1, 5):
                ty = 0 if dk <= 1 else 1
                slot = h * 6 + dk + 1
                nccol = ncf[:, ty * 4 + h:ty * 4 + h + 1]
                for ql in range(4):
                    dl = dk - ql
                    if -1 <= dl <= 1:
                        cs = slice(ql * 128, (ql + 1) * 128)
                        tk.op("act", lambda a, dl=dl, cs=cs: a.activation(
                            out=Dh[:, slot, cs], in_=Tt[:, h * 3 + dl + 1, :], func=AF.Identity, bias=nccol),
                            r=[("Tt", h, dl + 1), "ncf"], w=[("Dh", slot)])
                        tk.op("dve", lambda v, dl=dl, cs=cs: v.scalar_tensor_tensor(
                            out=Dl[:, slot, cs], in0=Tt[:, h * 3 + dl + 1, :], scalar=nccol, in1=Dh[:, slot, cs],
                            op0=ALU.add, op1=ALU.subtract),
                            r=[("Tt", h, dl + 1), "ncf", ("Dh", slot)], w=[("Dl", slot)])
        lq = lambda i: smc("lqk", i * 64, (i + 1) * 64)
        tk.op("dve", lambda v: v.tensor_tensor(out=lpr[:, 0:64], in0=lq(0), in1=lq(1), op=ALU.mult),
              r=["smalls"], w=["lpr"])
        tk.op("dve", lambda v: v.tensor_tensor(out=lpr[:, 64:128], in0=lq(2), in1=lq(3), op=ALU.mult),
              r=["smalls", "lpr"], w=["lpr"])
        for i in range(2):
            tk.op("act", lambda a, i=i: a.activation(out=junk3[:, 0:64], in_=lpr[:, i * 64:(i + 1) * 64], func=AF.Copy,
                                                     accum_out=lsum[:, i:i + 1]), r=["lpr"], w=["junk3", ("lsum", i)])
        tk.op("act", lambda a: a.activation(out=lexp[:], in_=lsum[:], func=AF.Exp), r=[("lsum", 0), ("lsum", 1)],
              w=["lexp"])
        tk.op("dve", lambda v: v.scalar_tensor_tensor(out=nlam[:], in0=lexp[:, 1:2], scalar=-LAM_INIT, in1=lexp[:, 0:1],
                                                      op0=ALU.add, op1=ALU.subtract), r=["lexp"], w=["nlam"])
        tk.op("dve", lambda v: v.tensor_scalar(out=gos[:], in0=smc("go"), scalar1=1.0 - LAM_INIT, scalar2=None,
                                               op0=ALU.mult), r=["smalls"], w=["gos"])

        pairs = [(h, qg, kc) for h in range(4) for qg in range(NG) for kc in range(NT)]
        NP = len(pairs)

        def emit_qk(n):
            h, qg, kc = pairs[n]
            i = h % 2
            b = n % 2
            dk = kc - 4 * qg
            near = -1 <= dk <= 4
            for j in range(2):
                osl = sps[b][:, j * 512:(j + 1) * 512]
                tk.op("pe", lambda pe: pe.matmul(osl, lhsT=kh[i][j * 64:(j + 1) * 64, kc * 128:(kc + 1) * 128],
                                                 rhs=qh[i][j * 64:(j + 1) * 64, qg * 512:(qg + 1) * 512],
                                                 start=True, stop=not near),
                      r=[("kh", i), ("qh", i)], w=[("sps", b)], inc=(j == 1 and not near))
            if near:
                slot = h * 6 + dk + 1
                qls = [ql for ql in range(4) if -1 <= dk - ql <= 1]
                c0, c1 = qls[0] * 128, (qls[-1] + 1) * 128
                for j in range(2):
                    osl = sps[b][:, j * 512 + c0:j * 512 + c1]
                    tk.op("pe", lambda pe: pe.matmul(osl, lhsT=ident_b[:], rhs=Dh[:, slot, c0:c1], start=False,
                                                     stop=False), r=["ident_b", ("Dh", slot)], w=[("sps", b)], inc=False)
                    tk.op("pe", lambda pe: pe.matmul(osl, lhsT=ident_b[:], rhs=Dl[:, slot, c0:c1], start=False,
                                                     stop=True), r=["ident_b", ("Dl", slot)], w=[("sps", b)],
                          inc=(j == 1))

        def emit_exp(n):
            h, qg, kc = pairs[n]
            b = n % 2
            ty = 0 if kc <= 4 * qg + 1 else 1
            tk.op("act", lambda a: a.activation(out=PT[n % 3][:], in_=sps[b][:], func=AF.Exp,
                                                bias=smc("rb", (15 + 16 * ty) * 4 + h)),
                  r=[("sps", b), "smalls"], w=[("PT", n % 3)])

        def emit_pv(n):
            h, qg, kc = pairs[n]
            i = h % 2
            for j in range(2):
                for ql in range(4):
                    a_ = j * 4 + ql
                    bank, off = a_ // 3, (a_ % 3) * 160
                    tk.op("pe", lambda pe, ql=ql, bank=bank, off=off, j=j: pe.matmul(
                        oacc[bank][:, off:off + 129], lhsT=PT[n % 3][:, j * 512 + ql * 128:j * 512 + (ql + 1) * 128],
                        rhs=vh[i][:, kc, :], start=(kc == 0 and a_ % 3 == 0), stop=(kc == NT - 1),
                        skip_group_check=True),
                        r=[("PT", n % 3), ("vh", i), ("vh1", i)], w=[("oacc", bank)], inc=(j == 1 and ql == 3))
            if kc == NT - 1:
                combine(h, qg)

        pending = []

        def combine(h, qg):
            for bank in range(3):
                na = 3 if bank < 2 else 2
                tk.op("dve", lambda v, bank=bank, na=na: v.tensor_copy(
                    ocp[:, bank, 0:na, 0:129],
                    oacc[bank][:, 0:480].rearrange("p (a c) -> p a c", c=160)[:, 0:na, 0:129]),
                    r=[("oacc", bank)], w=[("ocp", bank)])
            ock = [("ocp", bk) for bk in range(3)]
            tk.op("dve", lambda v: v.reciprocal(out=rz[:].rearrange("p (a b) -> p a b", b=3), in_=ocp[:, :, :, 128]),
                  r=ock, w=["rz"])
            tk.op("dve", lambda v: v.tensor_scalar(out=rz[:, 4:8], in0=rz[:, 4:8], scalar1=nlam[:, 0:1], scalar2=None,
                                                   op0=ALU.mult), r=["rz", "nlam"], w=["rz"])
            for ql in range(4):
                a0, a1 = ql, 4 + ql
                tk.op("dve", lambda v: v.tensor_scalar(out=o0[0][:], in0=ocp[:, a0 // 3, a0 % 3, 0:128],
                                                       scalar1=rz[:, a0:a0 + 1], scalar2=None, op0=ALU.mult),
                      r=ock + ["rz"], w=[("o0", 0)])
                tk.op("dve", lambda v: v.scalar_tensor_tensor(out=oo4[:, ql, :], in0=ocp[:, a1 // 3, a1 % 3, 0:128],
                                                              scalar=rz[:, a1:a1 + 1], in1=o0[0][:],
                                                              op0=ALU.mult, op1=ALU.add),
                      r=ock + ["rz", ("o0", 0)], w=[("oo4", ql)])
                tk.op("dve", lambda v: v.scalar_tensor_tensor(out=junk3[:], in0=oo4[:, ql, :], scalar=1.0,
                                                              in1=oo4[:, ql, :], op0=ALU.mult, op1=ALU.mult,
                                                              accum_out=oss[:, ql:ql + 1]),
                      r=[("oo4", ql)], w=["junk3", ("oss", ql)])
            ossk = [("oss", q) for q in range(4)]
            tk.op("pool", lambda p: p.tensor_scalar(out=oms[:], in0=oss[:], scalar1=1.0 / 128, scalar2=EPS,
                                                    op0=ALU.mult, op1=ALU.add), r=ossk, w=["oms"])
            tk.op("pool", lambda p: p.tensor_tensor(out=orstd[:], in0=oms[:], in1=mhalf[:, 0:4], op=ALU.pow),
                  r=["oms", "mhalf"], w=["orstd"])
            for ql in range(4):
                tk.op("dve", lambda v: v.scalar_tensor_tensor(out=yb4[:, ql, :], in0=oo4[:, ql, :],
                                                              scalar=orstd[:, ql:ql + 1], in1=gos[:],
                                                              op0=ALU.mult, op1=ALU.mult),
                      r=[("oo4", ql), "orstd", "gos"], w=[("yb4", ql)])
            pending.append((h, qg))

        def combine_b(h, qg):
            for ql in range(4):
                tk.op("pe", lambda pe: pe.transpose(tpo[:, ql, :], yb4[:, ql, :], ident_b[:]),
                      r=[("yb4", ql), "ident_b"], w=["tpo"], inc=(ql == 3))
            ysl = (h * NG + qg) % 2
            tk.op("dve", lambda v: v.tensor_copy(yst[ysl][:], tpo[:, 0:4, :]), r=["tpo"], w=[("yst", ysl)])
            tk.dma("sp", yT_d[4 + h, :, qg * 512:(qg + 1) * 512], yst[ysl][:], r=[("yst", ysl)],
                   w=[("yT_d", 4 + h, qg)], stream=f"yst{ysl}")

        emit_qk(0)
        emit_exp(0)
        emit_qk(1)
        emit_exp(1)
        for n in range(NP):
            h, qg, kc = pairs[n]
            if qg == 0 and kc == 0 and h + 1 < 4:
                load_head(h + 1)
            if n + 2 < NP:
                emit_qk(n + 2)
                emit_exp(n + 2)
            emit_pv(n)
            if kc == 8 and pending:
                combine_b(*pending.pop(0))
        while pending:
            combine_b(*pending.pop(0))
        tk.barrier()
    if stop_after == "p3":
        return finish(B, out_d, [])

    moe = ExitStack()
    wbuf = [[sb(moe, nc, f"wexp{i}_{m}", [128, 8, D], BF16) for m in range(3)] for i in range(2)]
    wsrc = (w1_d, w3_d, w2_d)

    def load_expert(e):
        for m in range(3):
            v = wsrc[m][e].rearrange("(kc p) n -> p kc n", p=128)
            for hh in range(2):
                tk.dma("pool", wbuf[e % 2][m][:, hh * 4:(hh + 1) * 4, :], v[:, hh * 4:(hh + 1) * 4, :],
                       w=[("wexp", e % 2, m, hh)], stream=f"we{e % 2}{m}{hh}")

    with ExitStack() as ph:
        wo_b = sb(ph, nc, "wo_b", [128, 8, D], BF16)
        wr_b = sb(ph, nc, "wr_b", [128, 128], BF16)
        yTg = [sb(ph, nc, f"yTg{i}", [128, 8, 512], BF16) for i in range(2)]
        xt = [sb(ph, nc, f"xt4_{i}", [128, D], F32) for i in range(3)]
        tmp = [sb(ph, nc, f"tmp4_{i}", [128, D], F32) for i in range(2)]
        x1 = [sb(ph, nc, f"x1_{i}", [128, D], F32) for i in range(2)]
        h2f = [sb(ph, nc, f"h2f{i}", [128, D], F32) for i in range(2)]
        h2b = [sb(ph, nc, f"h2b{i}", [128, D], BF16) for i in range(2)]
        h2T = [sb(ph, nc, f"h2T{i}", [128, 8, 128], BF16) for i in range(2)]
        junk = sb(ph, nc, "junk4", [128, D], BF16)
        ss = sb(ph, nc, "ss4", [128, NT], F32)
        ms = sb(ph, nc, "ms4", [128, NT], F32)
        rstd = sb(ph, nc, "rstd4", [128, NT], F32)
        mx = sb(ph, nc, "mx4", [128, NT], F32)
        esum = sb(ph, nc, "esum4", [128, NT], F32)
        resum = sb(ph, nc, "resum4", [128, NT], F32)
        eaff = [sb(ph, nc, f"eaff{i}", [128, NE], F32) for i in range(2)]
        mix = [[ps(ph, nc, f"mix{i}_{hh}", [128, 512], F32) for hh in range(2)] for i in range(2)]
        tp = [ps(ph, nc, f"tp4_{i}", [128, 8, 128], BF16) for i in range(2)]
        lg = [ps(ph, nc, f"lg{i}", [128, 512], F32) for i in range(2)]
        wo_v = wout_d.rearrange("(kc p) n -> p kc n", p=128)
        for hh in range(2):
            tk.dma("pool", wo_b[:, hh * 4:(hh + 1) * 4, :], wo_v[:, hh * 4:(hh + 1) * 4, :], w=[("wo_b", hh)],
                   stream=f"wo{hh}")
        tk.op("dve", lambda v: v.tensor_copy(wr_b[:], smc("wr")), r=["smalls"], w=["wr_b"])
        for kc in range(8):
            tk.op("dve", lambda v, kc=kc: v.tensor_tensor(out=wo_b[:, kc, :], in0=wo_b[:, kc, :], in1=g1bc[:],
                                                          op=ALU.mult), r=[("wo_b", kc // 4), ("gbc", 0)],
                  w=[("wo_b", kc // 4)])
        load_expert(0)
        load_expert(1)

        def load_g(g):
            tk.dma("sp", yTg[g % 2][:], yT_d[:, :, g * 512:(g + 1) * 512].rearrange("k p n -> p k n"),
                   w=[("yTg", g % 2, kc) for kc in range(8)], stream=f"yg{g % 2}")

        def load_x4(t):
            tk.dma("sp", xt[t % 3][:], x_d[t * 128:(t + 1) * 128, :], w=[("xt4", t % 3)], stream=f"x4{t % 3}")

        load_g(0)
        load_x4(0)
        load_x4(1)

        def p4_s0(t):
            g, tl = t // 4, t % 4
            i2 = t % 2
            if tl == 0 and g + 1 < NG:
                load_g(g + 1)
            if t + 2 < NT:
                load_x4(t + 2)
            for hh in range(2):
                for kc in range(8):
                    tk.op("pe", lambda pe, hh=hh, kc=kc: pe.matmul(
                        mix[i2][hh][:], lhsT=yTg[g % 2][:, kc, tl * 128:(tl + 1) * 128],
                        rhs=wo_b[:, kc, hh * 512:(hh + 1) * 512], start=(kc == 0), stop=(kc == 7)),
                        r=[("yTg", g % 2, kc), ("wo_b", kc // 4)], w=[("mix", i2, hh)], inc=(kc == 7))
            for hh in range(2):
                cs = slice(hh * 512, (hh + 1) * 512)
                tk.op("dve", lambda v, hh=hh, cs=cs: v.tensor_tensor(out=x1[i2][:, cs], in0=mix[i2][hh][:],
                                                                     in1=xt[t % 3][:, cs], op=ALU.add),
                      r=[("mix", i2, hh), ("xt4", t % 3)], w=[("x1", i2)])
            tk.dma("sp", out_d[t * 128:(t + 1) * 128, :], x1[i2][:], r=[("x1", i2)], w=[("out_d", t)],
                   stream=f"ox{i2}")
            tk.op("act", lambda a: a.activation(out=junk[:], in_=x1[i2][:], func=AF.Square, accum_out=ss[:, t:t + 1]),
                  r=[("x1", i2)], w=["junk4", ("ss4", t)])
            tk.op("act", lambda a: a.activation(out=ms[:, t:t + 1], in_=ss[:, t:t + 1], func=AF.Ln, scale=1.0 / D,
                                                bias=epsc[:, 0:1]), r=[("ss4", t), "epsc"], w=[("ms4", t)])
            tk.op("act", lambda a: a.activation(out=rstd[:, t:t + 1], in_=ms[:, t:t + 1], func=AF.Exp, scale=-0.5),
                  r=[("ms4", t)], w=[("rstd4", t)])

        def p4_s1(t):
            i2 = t % 2
            tk.op("dve", lambda v: v.scalar_tensor_tensor(out=h2f[i2][:], in0=x1[i2][:], scalar=rstd[:, t:t + 1],
                                                          in1=A2bc[:], op0=ALU.mult, op1=ALU.mult),
                  r=[("x1", i2), ("rstd4", t), ("gbc", 2)], w=[("h2f", i2)])
            tk.op("dve", lambda p: p.tensor_tensor(out=h2b[i2][:], in0=h2f[i2][:], in1=B2bc[:], op=ALU.add),
                  r=[("h2f", i2), ("gbc", 3)], w=[("h2b", i2)])
            tk.dma("sp", h2_d[t * 128:(t + 1) * 128, :], h2b[i2][:], r=[("h2b", i2)], w=[("h2_d", t)],
                   stream=f"oh{i2}")

        def p4_s1b(t):
            i2 = t % 2
            for kc in range(8):
                tk.op("pe", lambda pe, kc=kc: pe.transpose(tp[i2][:, kc, :], h2b[i2][:, kc * 128:(kc + 1) * 128],
                                                           ident_b[:]),
                      r=[("h2b", i2), "ident_b"], w=[("tp4", i2)], inc=(kc == 7))
            tk.op("act", lambda a: a.copy(out=h2T[i2][:], in_=tp[i2][:]), r=[("tp4", i2)], w=[("h2T", i2)])

        def p4_s2(t):
            i2 = t % 2
            for kc in range(8):
                tk.op("pe", lambda pe, kc=kc: pe.matmul(lg[i2][:, 0:NE], lhsT=h2T[i2][:, kc, :],
                                                        rhs=wr_b[:, kc * NE:(kc + 1) * NE], start=(kc == 0),
                                                        stop=(kc == 7)),
                      r=[("h2T", i2), "wr_b"], w=[("lg", i2)], inc=(kc == 7))
            tk.op("dve", lambda v: v.reduce_max(out=mx[:, t:t + 1], in_=lg[i2][:, 0:NE], axis=AX.X),
                  r=[("lg", i2)], w=[("mx4", t)])
            tk.op("dve", lambda v: v.tensor_scalar(out=mx[:, t:t + 1], in0=mx[:, t:t + 1], scalar1=-1.0, scalar2=None,
                                                   op0=ALU.mult), r=[("mx4", t)], w=[("mx4", t)])
            tk.op("act", lambda a: a.activation(out=eaff[i2][:], in_=lg[i2][:, 0:NE], func=AF.Exp,
                                                bias=mx[:, t:t + 1], accum_out=esum[:, t:t + 1]),
                  r=[("lg", i2), ("mx4", t)], w=[("eaff", i2), ("esum4", t)])
            tk.op("dve", lambda v: v.reciprocal(out=resum[:, t:t + 1], in_=esum[:, t:t + 1]),
                  r=[("esum4", t)], w=[("resum4", t)])
            tk.op("dve", lambda v: v.tensor_scalar(out=affT[:, t, :], in0=eaff[i2][:], scalar1=resum[:, t:t + 1],
                                                   scalar2=None, op0=ALU.mult),
                  r=[("eaff", i2), ("resum4", t)], w=[("affT", t)])

        for slot in range(NT + 3):
            if slot < NT:
                p4_s0(slot)
            if 0 <= slot - 1 < NT:
                p4_s1(slot - 1)
            if 0 <= slot - 2 < NT:
                p4_s1b(slot - 2)
            if 0 <= slot - 3 < NT:
                p4_s2(slot - 3)
        tk.barrier()
    if stop_after == "p4":
        moe.close()
        return finish(B, out_d, [])

    idx_i = sb(moe, nc, "idx_i", [128, 4 * NE], I32)
    gsel = sb(moe, nc, "gsel", [128, 4 * NE], F32)
    L4 = sb(moe, nc, "L4", [128, 4, NT, NE], BF16)
    pm = sb(moe, nc, "pm", [128, NT, NE], F32)
    iota1 = sb(moe, nc, "iota1_sb", [128, 512], F32)
    oh = [sb(moe, nc, f"oh{i}", [128, 512], BF16) for i in range(8)]
    Rs = sb(moe, nc, "Rs", [128, 16], F32)
    idxf = sb(moe, nc, "idxf", [128, 4], F32)
    R_ps = ps(moe, nc, "R_ps", [128, 2, 256], F32)
    with ExitStack() as ph:
        affE = sb(ph, nc, "affE", [128, 512], F32)
        junkE = sb(ph, nc, "junkE", [128, 512], BF16)
        maskE = sb(ph, nc, "maskE", [128, 512], BF16)
        gsum = sb(ph, nc, "gsum_sb", [128, 128], F32)
        lo = sb(ph, nc, "lo", [128, 1], F32)
        mid = sb(ph, nc, "mid", [128, 1], F32)
        cntt = sb(ph, nc, "cntt", [128, 1], F32)
        ge = sb(ph, nc, "ge", [128, 1], F32)
        mask_tm = sb(ph, nc, "mask_tm", [128, NT, NE], BF16)
        ut_b = sb(ph, nc, "ut_b_sb", [128, 128], BF16)
        ones_b = sb(ph, nc, "ones_b_sb", [128, 128], BF16)
        totE = sb(ph, nc, "totE", [128, NE, NT], F32)
        flg = sb(ph, nc, "flg", [128, NE, NT], F32)
        cumE = sb(ph, nc, "cumE", [128, NE, NT], F32)
        p1 = sb(ph, nc, "p1", [128, NT, NE], F32)
        tpa = ps(ph, nc, "tpa", [128, 512], F32)
        cnt_ps = ps(ph, nc, "cnt_ps", [128, 512], F32)
        tpm = ps(ph, nc, "tpm", [128, NT, NE], BF16)
        pfx_ps = ps(ph, nc, "pfx_ps", [128, 512], F32)
        tot_ps = ps(ph, nc, "tot_ps", [128, 512], F32)
        tk.dma("sp", ut_b[:], cst["ut_b"], w=["ut_b"], stream="c0")
        tk.dma("sp", ones_b[:], cst["ones_b"], w=["ones_b"], stream="c1")
        tk.dma("sp", iota1[:], cst["iota1"], w=["iota1"], stream="c2")
        tk.dma("sp", L4[:, 0:2, :, :], cst["tokab"], w=[("L4", 0)], stream="c3")
        tk.dma("sp", gsum[:], cst["gsum"], w=["gsum"], stream="c4")
        tk.op("dve", lambda p: p.memset(flg[:], 1.0), w=["flg"])
        tk.op("dve", lambda p: p.memset(flg[:, :, 0:1], 0.0), r=["flg"], w=["flg"])
        tk.op("dve", lambda p: p.memset(lo[:], 0.0), w=["lo"])
        tk.op("act", lambda a: a.copy(out=L4[:, 2, :, :], in_=affT[:]), r=[("affT", t) for t in range(NT)],
              w=[("L4", 2)])
        tk.op("dve", lambda p: p.tensor_tensor(out=L4[:, 3, :, :], in0=affT[:], in1=L4[:, 2, :, :], op=ALU.subtract),
              r=[("affT", t) for t in range(NT)] + [("L4", 2)], w=[("L4", 3)])
        for q4 in range(4):
            tk.op("pe", lambda pe, q4=q4: pe.transpose(
                tpa[:, q4 * 128:(q4 + 1) * 128], affT[:, q4 * 8:(q4 + 1) * 8, :].rearrange("p t e -> p (t e)"),
                ident_f[:]), r=[("affT", t) for t in range(q4 * 8, q4 * 8 + 8)] + ["ident_f"], w=["tpa"])
        tk.op("act", lambda a: a.copy(out=affE[:], in_=tpa[:]), r=["tpa"], w=["affE"])
        for it in range(30):
            wdt = 2.0 ** -(it + 1)
            tk.op("dve", lambda v: v.tensor_scalar(out=mid[:], in0=lo[:], scalar1=wdt, scalar2=None, op0=ALU.add),
                  r=["lo"], w=["mid"])
            tk.op("dve", lambda v: v.tensor_scalar(out=junkE[:], in0=affE[:], scalar1=mid[:, 0:1], scalar2=None,
                                                   op0=ALU.is_gt, op1=ALU.add, accum_out=cntt[:, 0:1]),
                  r=["affE", "mid"], w=["junkE", "cntt"])
            tk.op("pe", lambda pe: pe.matmul(cnt_ps[:, 0:1], lhsT=gsum[:], rhs=cntt[:, 0:1], start=True, stop=True),
                  r=["gsum", "cntt"], w=["cnt_ps"])
            tk.op("dve", lambda v: v.tensor_scalar(out=ge[:], in0=cnt_ps[:, 0:1], scalar1=float(CAP), scalar2=None,
                                                   op0=ALU.is_ge), r=["cnt_ps"], w=["ge"])
            tk.op("dve", lambda v: v.scalar_tensor_tensor(out=lo[:], in0=mid[:], scalar=ge[:, 0:1], in1=lo[:],
                                                          op0=ALU.mult, op1=ALU.max), r=["mid", "ge", "lo"], w=["lo"])
        tk.op("dve", lambda v: v.tensor_scalar(out=maskE[:], in0=affE[:], scalar1=lo[:, 0:1], scalar2=None,
                                               op0=ALU.is_gt), r=["affE", "lo"], w=["maskE"])
        for q4 in range(4):
            tk.op("pe", lambda pe, q4=q4: pe.transpose(tpm[:, q4 * 8:(q4 + 1) * 8, :].rearrange("p t e -> p (t e)"),
                                                       maskE[:, q4 * 128:(q4 + 1) * 128], ident_b[:]),
                  r=["maskE", "ident_b"], w=["tpm"], inc=(q4 == 3))
        tk.op("act", lambda a: a.copy(out=mask_tm[:], in_=tpm[:]), r=["tpm"], w=["mask_tm"])
        mflat = mask_tm[:].rearrange("p t e -> p (t e)")
        tk.op("pe", lambda pe: pe.matmul(pfx_ps[:], lhsT=ut_b[:], rhs=mflat, start=True, stop=True),
              r=["ut_b", "mask_tm"], w=["pfx_ps"])
        tk.op("pe", lambda pe: pe.matmul(tot_ps[:], lhsT=ones_b[:], rhs=mflat, start=True, stop=True),
              r=["ones_b", "mask_tm"], w=["tot_ps"])
        tk.op("dve", lambda v: v.tensor_copy(totE[:].rearrange("p e t -> p t e"),
                                             tot_ps[:].rearrange("p (t e) -> p t e", e=NE)),
              r=["tot_ps"], w=["totE"])
        tk.op("dve", lambda v: v.tensor_tensor_scan(out=cumE[:].rearrange("p e t -> p (e t)"),
                                                    data0=flg[:].rearrange("p e t -> p (e t)"),
                                                    data1=totE[:].rearrange("p e t -> p (e t)"), initial=0.0,
                                                    op0=ALU.mult, op1=ALU.add), r=["flg", "totE"], w=["cumE"])
        tk.op("dve", lambda v: v.tensor_tensor(out=cumE[:], in0=cumE[:], in1=totE[:], op=ALU.subtract),
              r=["cumE", "totE"], w=["cumE"])
        tk.op("dve", lambda v: v.tensor_tensor(out=p1[:], in0=pfx_ps[:].rearrange("p (t e) -> p t e", e=NE),
                                               in1=cumE[:].rearrange("p e t -> p t e"), op=ALU.add),
              r=["pfx_ps", "cumE"], w=["p1"])
        tk.op("dve", lambda v: v.tensor_tensor(out=pm[:], in0=p1[:], in1=mask_tm[:], op=ALU.mult),
              r=["p1", "mask_tm"], w=["pm"])
        tk.barrier()

    ohc = [0]

    def build_idx_gen(e):
        rb_ = 0
        base = ohc[0]
        ohc[0] += NT

        def onehot(t):
            o = (base + t) % 8
            tk.op("dve", lambda v: v.tensor_scalar(out=oh[o][:], in0=iota1[:], scalar1=pm[:, t, e:e + 1],
                                                   scalar2=None, op0=ALU.is_equal),
                  r=["iota1", "pm"], w=[("oh", o)])

        LA = 5
        for t in range(LA):
            onehot(t)
        yield
        for t in range(NT):
            o = (base + t) % 8
            if t + LA < NT:
                onehot(t + LA)
            for j in range(4):
                tk.op("pe", lambda pe, t=t, o=o, j=j: pe.matmul(
                    R_ps[:, rb_, j * 4:(j + 1) * 4], lhsT=oh[o][:, j * 128:(j + 1) * 128], rhs=L4[:, :, t, e],
                    start=(t == 0 and j == 0), stop=(t == NT - 1), skip_group_check=True),
                    r=[("oh", o), ("L4", 0), ("L4", 2), ("L4", 3)], w=["R_ps"], inc=(j == 3))
            if t < NT - 1:
                yield
        tk.op("dve", lambda v: v.tensor_copy(Rs[:], R_ps[:, rb_, 0:16]), r=["R_ps"], w=["Rs"])
        Rv = Rs[:].rearrange("p (j c) -> p j c", c=4)
        tk.op("dve", lambda v: v.scalar_tensor_tensor(out=idxf[:], in0=Rv[:, :, 0], scalar=64.0, in1=Rv[:, :, 1],
                                                      op0=ALU.mult, op1=ALU.add), r=["Rs"], w=["idxf"])
        tk.op("dve", lambda v: v.tensor_copy(idx_i[:, e * 4:(e + 1) * 4], idxf[:]), r=["idxf"], w=[("idx", e)])
        tk.op("dve", lambda v: v.tensor_tensor(out=gsel[:, e * 4:(e + 1) * 4], in0=Rv[:, :, 2], in1=Rv[:, :, 3],
                                               op=ALU.add), r=["Rs"], w=[("gsel", e)])
        yield

    def build_idx(e):
        for _ in build_idx_gen(e):
            pass

    if stop_after == "p5":
        dbg_idx = B.dram_scratch("dbg_idx", [128, 4 * NE], I32)
        dbg_g = B.dram_scratch("dbg_g", [128, 4 * NE], F32)
        dbg_pm = B.dram_scratch("dbg_pm", [128, NT * NE], F32)
        for e in range(NE):
            build_idx(e)
        tk.dma("sp", dbg_idx, idx_i[:], r=[("idx", e) for e in range(NE)], w=["dbg_idx"], stream="c0")
        tk.dma("sp", dbg_g, gsel[:], r=[("gsel", e) for e in range(NE)], w=["dbg_g"], stream="c1")
        tk.dma("sp", dbg_pm, pm[:].rearrange("p t e -> p (t e)"), r=["pm"], w=["dbg_pm"], stream="c2")
        moe.close()
        return finish(B, out_d, [])

    with ExitStack() as ph:
        xe = [sb(ph, nc, f"xe{i}", [128, D], BF16) for i in range(8)]
        xeT = [sb(ph, nc, f"xeT{i}", [128, 8, 512], BF16) for i in range(2)]
        hT = sb(ph, nc, "hT6", [128, 8, 512], BF16)
        thh = [sb(ph, nc, f"thh{i}", [128, 512], F32) for i in range(2)]
        t1h = [sb(ph, nc, f"t1h{i}", [128, 512], F32) for i in range(2)]
        ysc = [sb(ph, nc, f"ysc{i}", [128, D], F32) for i in range(4)]
        tpx = ps(ph, nc, "tpx", [128, 2, 4, 128], BF16)
        pa = [ps(ph, nc, f"pa{i}", [128, 512], F32) for i in range(2)]
        pb = [ps(ph, nc, f"pb{i}", [128, 512], F32) for i in range(2)]
        py = [ps(ph, nc, f"py{i}", [128, 512], F32) for i in range(2)]
        def load_w(e, ms_):
            for m in ms_:
                v = wsrc[m][e].rearrange("(kc p) n -> p kc n", p=128)
                for hh in range(2):
                    tk.dma("pool", wbuf[e % 2][m][:, hh * 4:(hh + 1) * 4, :], v[:, hh * 4:(hh + 1) * 4, :],
                           w=[("wexp", e % 2, m, hh)], stream=f"we{e % 2}{m}{hh}")

        def gather(e):
            for j in range(4):
                tk.dma("pool", xe[(e % 2) * 4 + j][:], h2_d, r=[("idx", e)], w=[("xe", e % 2, j)],
                       stream=f"xg{e % 2}{j}",
                       indirect=dict(out_offset=None,
                                     in_offset=bass.IndirectOffsetOnAxis(ap=idx_i[:, e * 4 + j:e * 4 + j + 1], axis=0)))

        def transpose_kc(e, kc):
            for j in range(4):
                tk.op("pe", lambda pe, j=j: pe.transpose(
                    tpx[:, 0, j, :], xe[(e % 2) * 4 + j][:, kc * 128:(kc + 1) * 128], ident_b[:]),
                    r=[("xe", e % 2, j), "ident_b"], w=["tpx"], inc=(j == 3))
            if kc % 2 == 0:
                tk.op("act", lambda a: a.copy(out=xeT[e % 2][:, kc, :], in_=tpx[:, 0, :, :]),
                      r=["tpx"], w=[("xeT", e % 2, kc)])
            else:
                tk.op("dve", lambda v: v.tensor_copy(xeT[e % 2][:, kc, :], tpx[:, 0, :, :]),
                      r=["tpx"], w=[("xeT", e % 2, kc)])

        def transposes(e):
            for kc in range(8):
                transpose_kc(e, kc)

        build_idx(0)
        gather(0)
        build_idx(1)
        transposes(0)
        yc = [0]
        for e in range(NE):
            wb = wbuf[e % 2]
            wk = lambda m: [("wexp", e % 2, m, 0), ("wexp", e % 2, m, 1)]
            if e + 1 < NE:
                gather(e + 1)
            xk = [("xeT", e % 2, kc) for kc in range(8)]
            bgen = build_idx_gen(e + 2) if e + 2 < NE else iter(())

            def bstep(k):
                for _ in range(k):
                    next(bgen, None)

            for fc in range(8):
                i2 = fc % 2
                for kc in range(8):
                    tk.op("pe", lambda pe, kc=kc: pe.matmul(pa[i2][:], lhsT=wb[0][:, kc, fc * 128:(fc + 1) * 128],
                                                            rhs=xeT[e % 2][:, kc, :], start=(kc == 0), stop=(kc == 7)),
                          r=xk + wk(0), w=[("pa", i2)], inc=(kc == 7))
                for kc in range(8):
                    tk.op("pe", lambda pe, kc=kc: pe.matmul(pb[i2][:], lhsT=wb[1][:, kc, fc * 128:(fc + 1) * 128],
                                                            rhs=xeT[e % 2][:, kc, :], start=(kc == 0), stop=(kc == 7)),
                          r=xk + wk(1), w=[("pb", i2)], inc=(kc == 7))
                tk.op("act", lambda a: a.activation(out=thh[i2][:], in_=pa[i2][:], func=AF.Tanh, scale=0.5),
                      r=[("pa", i2)], w=[("thh", i2)])
                tk.op("dve", lambda v: v.scalar_tensor_tensor(out=t1h[i2][:], in0=thh[i2][:], scalar=1.0, in1=pa[i2][:],
                                                              op0=ALU.add, op1=ALU.mult),
                      r=[("thh", i2), ("pa", i2)], w=[("t1h", i2)])
                tk.op("dve", lambda v: v.scalar_tensor_tensor(out=hT[:, fc, :], in0=t1h[i2][:], scalar=0.5,
                                                              in1=pb[i2][:], op0=ALU.mult, op1=ALU.mult),
                      r=[("t1h", i2), ("pb", i2)], w=[("hT6", fc)])
                bstep(2)
            if e + 2 < NE:
                load_w(e + 2, (0, 1))
            hk = [("hT6", fc) for fc in range(8)]
            for j in range(4):
                ys_ = yc[0] % 4
                yc[0] += 1
                for hh in range(2):
                    i2 = hh
                    for fc in range(8):
                        tk.op("pe", lambda pe, fc=fc: pe.matmul(py[i2][:], lhsT=hT[:, fc, j * 128:(j + 1) * 128],
                                                                rhs=wb[2][:, fc, hh * 512:(hh + 1) * 512],
                                                                start=(fc == 0), stop=(fc == 7)),
                              r=hk + wk(2), w=[("py", i2)], inc=(fc == 7))
                    tk.op("dve", lambda v: v.scalar_tensor_tensor(
                        out=ysc[ys_][:, hh * 512:(hh + 1) * 512], in0=py[i2][:], scalar=gsel[:, e * 4 + j:e * 4 + j + 1],
                        in1=g2bc[:, hh * 512:(hh + 1) * 512], op0=ALU.mult, op1=ALU.mult),
                        r=[("py", i2), ("gsel", e), ("gbc", 1)], w=[("ysc", ys_, hh)])
                    if e + 1 < NE:
                        transpose_kc(e + 1, j * 2 + hh)
                prev = [("outacc", e - 1, jj) for jj in range(4)] if e > 0 else []
                tk.dma("pool", out_d, ysc[ys_][:], r=[("ysc", ys_, 0), ("ysc", ys_, 1), ("idx", e)] + prev,
                       w=[("outacc", e, j)], stream=f"sc{j}",
                       indirect=dict(out_offset=bass.IndirectOffsetOnAxis(ap=idx_i[:, e * 4 + j:e * 4 + j + 1], axis=0),
                                     in_offset=None, compute_op=ALU.add))
                bstep(4)
            bstep(NT + 1)
            if e + 2 < NE:
                load_w(e + 2, (2,))
        tk.barrier()
    moe.close()
    return finish(B, out_d, [])


def finish(B, out_d, extra):
    tk = B.tk
    tk.wait_all("sp")
    tk.wait_all("pool")
    B.root.close()
    return B.nc


def make_in_maps(inputs):
    consts = host_consts()
    maps = []
    shared = {
        "w_mod": np.ascontiguousarray(inputs["w_mod"][0], dtype=np.float32),
        "w_in": np.ascontiguousarray(inputs["w_in"][0], dtype=np.float32),
        "lru_w_a": np.ascontiguousarray(inputs["lru_w_a"][0], dtype=np.float32),
        "lru_w_x": np.ascontiguousarray(inputs["lru_w_x"][0], dtype=np.float32),
        "w_out": np.ascontiguousarray(inputs["w_out"][0], dtype=np.float32),
        "w1": np.ascontiguousarray(inputs["w1"][0], dtype=np.float32),
        "w3": np.ascontiguousarray(inputs["w3"][0], dtype=np.float32),
        "w2": np.ascontiguousarray(inputs["w2"][0], dtype=np.float32),
        "rel_bias": np.ascontiguousarray(np.repeat(np.asarray(inputs["rel_bias"], np.float32)[:, :, None], 128, axis=2)),
    }
    shared.update(consts)
    for b in range(8):
        m = dict(shared)
        m["x"] = np.ascontiguousarray(inputs["x"][b], dtype=np.float32)
        m["smalls"] = pack_smalls(inputs, b)
        maps.append(m)
    return maps


def kernel(**inputs):
    inputs = {k: np.asarray(v) for k, v in inputs.items()}
    nc = build()
    res = run_bass_kernel_spmd(nc, make_in_maps(inputs), core_ids=list(range(8)))
    return np.stack([np.asarray(r["out"], dtype=np.float32) for r in res.results], axis=0)
```

```python
import math
from contextlib import ExitStack

import numpy as np
import ml_dtypes

import concourse.bass as bass
import concourse.mybir as mybir
from concourse.bass_utils import run_bass_kernel_spmd

F32 = mybir.dt.float32
BF16 = mybir.dt.bfloat16
I32 = mybir.dt.int32
AF = mybir.ActivationFunctionType
ALU = mybir.AluOpType
AX = mybir.AxisListType

S = 4096
D = 1024
NT = 32
NG = 8
NE = 16
CAP = 512
EPS = 1e-6
LAM_INIT = 0.2
C1 = math.sqrt(2.0 / math.pi)

SM = {}
_off = 0
for _n, _w in [("c", 8), ("bmod", 48), ("g1", 8), ("g2", 8), ("convw", 16), ("convb", 4), ("ba", 8), ("bx", 8),
               ("lam", 8), ("gq", 1), ("gk", 1), ("lqk", 256), ("rb", 128), ("go", 128), ("wr", 128)]:
    SM[_n] = (_off, _off + _w)
    _off += _w
NS = _off


def _bucket_table():
    out = np.zeros(511, np.int64)
    for i in range(511):
        rel = i - 255
        ret = 16 if rel > 0 else 0
        n = abs(rel)
        nf = np.float32(max(n, 1))
        large = 8 + int(np.float32(np.float32(np.log(np.float32(nf / np.float32(8.0)))) / np.float32(math.log(16.0)))
                        * np.float32(8.0))
        large = min(large, 15)
        out[i] = ret + (n if n < 8 else large)
    return out


def host_consts():
    ident = np.eye(128, dtype=np.float32)
    b64 = np.zeros((128, 128), np.float32)
    b64[:64, :64] = 1
    b64[64:, 64:] = 1
    ut = np.triu(np.ones((128, 128), np.float32))
    bt = _bucket_table()
    ohrev = np.zeros((32, 511), np.float32)
    for i in range(511):
        ohrev[bt[(255 - i) + 255], i] = 1.0
    p = np.arange(128)[:, None, None]
    t = np.arange(32)[None, :, None]
    tok = np.broadcast_to(t * 128 + p, (128, 32, 16))
    tokab = np.stack([tok // 64, tok % 64], axis=1).astype(np.float32)
    iota1 = np.broadcast_to(np.arange(1, 513, dtype=np.float32)[None, :], (128, 512))
    pp = np.arange(128)
    gsum = (pp[:, None] % 16 == pp[None, :] % 16).astype(np.float32)
    return {
        "gsum": gsum,
        "tokab": np.ascontiguousarray(tokab).astype(ml_dtypes.bfloat16),
        "iota1": np.ascontiguousarray(iota1),
        "ident_f": ident,
        "ident_b": ident.astype(ml_dtypes.bfloat16),
        "ones_f": np.ones((128, 128), np.float32),
        "b64_b": b64.astype(ml_dtypes.bfloat16),
        "ut_b": ut.astype(ml_dtypes.bfloat16),
        "ones_b": np.ones((128, 128), ml_dtypes.bfloat16),
        "ohrev": ohrev,
    }


def pack_smalls(inp, b):
    sm = np.zeros((128, NS), np.float32)

    def colmaj(v, nchunk):
        return np.ascontiguousarray(np.asarray(v, np.float32).reshape(nchunk, 128).T)

    def put(name, arr):
        a, e = SM[name]
        sm[:, a:e] = arr

    put("c", colmaj(inp["c"][b], 8))
    put("bmod", colmaj(inp["b_mod"][0], 48))
    put("g1", colmaj(inp["g_norm1"][0], 8))
    put("g2", colmaj(inp["g_norm2"][0], 8))
    cw = np.asarray(inp["conv_w"][0], np.float32).reshape(4, 512)
    put("convw", np.ascontiguousarray(cw.reshape(4, 4, 128).transpose(2, 1, 0)).reshape(128, 16))
    put("convb", colmaj(inp["conv_b"][0], 4))
    for nm, key in (("ba", "lru_b_a"), ("bx", "lru_b_x"), ("lam", "lru_lambda")):
        v = np.asarray(inp[key][0], np.float32)
        put(nm, np.concatenate([colmaj(v[0], 4), colmaj(v[1], 4)], axis=1))
    put("gq", np.tile(np.asarray(inp["g_q"][0], np.float32), 2).reshape(128, 1))
    put("gk", np.tile(np.asarray(inp["g_k"][0], np.float32), 2).reshape(128, 1))
    put("lqk", np.tile(np.asarray(inp["lambda_qk"][0], np.float32).reshape(1, 256), (128, 1)))
    put("rb", np.tile(np.asarray(inp["rel_bias"], np.float32).reshape(1, 128), (128, 1)))
    put("go", np.tile(np.asarray(inp["g_attn_out"][0], np.float32).reshape(1, 128), (128, 1)))
    wr = np.asarray(inp["w_router"][0], np.float32)
    put("wr", np.ascontiguousarray(wr.reshape(8, 128, 16).transpose(1, 0, 2)).reshape(128, 128))
    return sm


class TK:
    ENG = ("pe", "act", "dve", "pool", "sp")

    def __init__(self, nc, ctx):
        self.nc = nc
        self.ctx = ctx
        self.eng = {"pe": nc.tensor, "act": nc.scalar, "dve": nc.vector, "pool": nc.gpsimd, "sp": nc.sync}
        self.semh = {}
        self.cnt = {}
        for e in self.ENG:
            self.semh[e] = ctx.enter_context(nc.semaphore("s_" + e))
            self.cnt[e] = 0
        self.waited = {e: {} for e in self.ENG}
        self.last_w = {}
        self.readers = {}

    def _wait(self, e, stamp):
        if stamp is None:
            return
        sk, val = stamp
        if sk == "pe" and e == "pe":
            return
        if sk == e:
            assert val <= self.cnt[e], ("self-wait on future inc", e, val, self.cnt[e])
        if self.waited[e].get(sk, 0) >= val:
            return
        self.eng[e].wait_ge(self.semh[sk], val)
        self.waited[e][sk] = val

    def _deps(self, e, r, w):
        need = {}

        def add(stamp):
            if stamp is not None:
                need[stamp[0]] = max(need.get(stamp[0], 0), stamp[1])

        for k in r:
            add(self.last_w.get(k))
        for k in w:
            add(self.last_w.get(k))
            for sk, val in self.readers.get(k, {}).items():
                add((sk, val))
        for sk, val in need.items():
            self._wait(e, (sk, val))

    def _record(self, stamp, r, w):
        sk, val = stamp
        for k in r:
            d = self.readers.setdefault(k, {})
            d[sk] = max(d.get(sk, 0), val)
        for k in w:
            self.last_w[k] = stamp
            self.readers[k] = {}

    def op(self, e, fn, r=(), w=(), inc=True):
        self._deps(e, r, w)
        ins = fn(self.eng[e])
        if inc:
            self.cnt[e] += 1
            ins.then_inc(self.semh[e], 1)
            stamp = (e, self.cnt[e])
        else:
            stamp = (e, self.cnt[e] + 1)
        self._record(stamp, r, w)
        return ins

    def dma(self, q, out, in_, r=(), w=(), stream="d", indirect=None, **kw):
        self._deps(q, r, w)
        sk = "d:" + stream
        if sk not in self.semh:
            self.semh[sk] = self.ctx.enter_context(self.nc.semaphore("s_" + stream))
            self.cnt[sk] = 0
        if indirect is None:
            ins = self.eng[q].dma_start(out=out, in_=in_, **kw)
        else:
            ins = self.eng[q].indirect_dma_start(out=out, in_=in_, **indirect, **kw)
        self.cnt[sk] += 16
        ins.then_inc(self.semh[sk], 16)
        self._record((sk, self.cnt[sk]), r, w)
        return ins

    def barrier(self):
        for e in self.ENG:
            for sk, c in self.cnt.items():
                if c > 0 and not (sk == "pe" and e == "pe"):
                    self._wait(e, (sk, c))
        self.last_w = {}
        self.readers = {}

    def wait_all(self, e):
        for sk, c in self.cnt.items():
            if sk != e and c > 0:
                self._wait(e, (sk, c))


class Bld:
    def __init__(self, debug=None):
        self.debug = debug
        self.nc = bass.Bass("TRN2", target_bir_lowering=False)
        self.root = ExitStack()
        self.tk = TK(self.nc, self.root)

    def dram_in(self, name, shape, dt):
        return self.nc.dram_tensor(name, list(shape), dt, kind="ExternalInput").ap()

    def dram_scratch(self, name, shape, dt):
        kind = "ExternalOutput" if (self.debug and name in self.debug) else "Internal"
        return self.nc.dram_tensor(name, list(shape), dt, kind=kind).ap()


def sb(ctx, nc, name, shape, dt):
    return ctx.enter_context(nc.sbuf_tensor(name, list(shape), dt))


def ps(ctx, nc, name, shape, dt):
    return ctx.enter_context(nc.psum_tensor(name, list(shape), dt))


def build(debug=None, stop_after=None, opts=()):
    B = Bld(debug)
    nc, tk = B.nc, B.tk
    root = B.root
    x_d = B.dram_in("x", [S, D], F32)
    sm_d = B.dram_in("smalls", [128, NS], F32)
    wmod_d = B.dram_in("w_mod", [D, 6 * D], F32)
    win_d = B.dram_in("w_in", [D, 2560], F32)
    lwa_d = B.dram_in("lru_w_a", [2, 8, 64, 64], F32)
    lwx_d = B.dram_in("lru_w_x", [2, 8, 64, 64], F32)
    wout_d = B.dram_in("w_out", [D, D], F32)
    w1_d = B.dram_in("w1", [NE, D, D], F32)
    w3_d = B.dram_in("w3", [NE, D, D], F32)
    w2_d = B.dram_in("w2", [NE, D, D], F32)
    rbk_d = B.dram_in("rel_bias", [32, 4, 128], F32)
    cst = {}
    for nm, shp, dt in [("ident_f", [128, 128], F32), ("ident_b", [128, 128], BF16), ("ones_f", [128, 128], F32),
                        ("b64_b", [128, 128], BF16), ("ut_b", [128, 128], BF16), ("ones_b", [128, 128], BF16),
                        ("ohrev", [32, 511], F32), ("tokab", [128, 2, 32, 16], BF16), ("iota1", [128, 512], F32),
                        ("gsum", [128, 128], F32)]:
        cst[nm] = B.dram_in(nm, shp, dt)
    out_d = nc.dram_tensor("out", [S, D], F32, kind="ExternalOutput").ap()
    xl_d = B.dram_scratch("xl_d", [4, 128, S], F32)
    gz_d = B.dram_scratch("gz_d", [4, 128, S], BF16)
    qT_d = B.dram_scratch("qT_d", [4, 128, S], BF16)
    kT_d = B.dram_scratch("kT_d", [4, 128, S], BF16)
    v_d = B.dram_scratch("v_d", [S, 512], BF16)
    yT_d = B.dram_scratch("yT_d", [8, 128, S], BF16)
    f_d = B.dram_scratch("f_d", [4, 128, 511], F32)
    h2_d = B.dram_scratch("h2_d", [S, D], BF16)

    smalls = sb(root, nc, "smalls_sb", [128, NS], F32)
    ident_f = sb(root, nc, "ident_f_sb", [128, 128], F32)
    ident_b = sb(root, nc, "ident_b_sb", [128, 128], BF16)
    ones_f = sb(root, nc, "ones_f_sb", [128, 128], F32)
    b64_b = sb(root, nc, "b64_b_sb", [128, 128], BF16)
    modT = sb(root, nc, "modT", [128, 48], F32)
    A1 = sb(root, nc, "A1", [128, 8], F32)
    A2 = sb(root, nc, "A2", [128, 8], F32)
    g1bc = sb(root, nc, "g1bc", [128, D], F32)
    g2bc = sb(root, nc, "g2bc", [128, D], F32)
    A2bc = sb(root, nc, "A2bc", [128, D], F32)
    B2bc = sb(root, nc, "B2bc", [128, D], F32)
    affT = sb(root, nc, "affT", [128, NT, NE], F32)
    epsc = sb(root, nc, "epsc", [128, 1], F32)
    mhalf = sb(root, nc, "mhalf", [128, 8], F32)

    def smc(name, i=None, j=None):
        a, e = SM[name]
        if i is None:
            return smalls[:, a:e]
        return smalls[:, a + i:a + (j if j is not None else i + 1)]

    tk.dma("sp", smalls[:], sm_d, w=["smalls"], stream="c0")
    tk.dma("sp", ident_f[:], cst["ident_f"], w=["ident_f"], stream="c1")
    tk.dma("sp", ident_b[:], cst["ident_b"], w=["ident_b"], stream="c2")
    tk.dma("sp", ones_f[:], cst["ones_f"], w=["ones_f"], stream="c3")
    tk.dma("sp", b64_b[:], cst["b64_b"], w=["b64_b"], stream="c4")
    tk.op("pool", lambda g: g.memset(mhalf[:], -0.5), w=["mhalf"])
    tk.op("pool", lambda g: g.memset(epsc[:], EPS), w=["epsc"])

    with ExitStack() as ph:
        win_b = sb(ph, nc, "win_b", [128, 8, 2560], BF16)
        xt = [sb(ph, nc, f"xt{i}", [128, D], F32) for i in range(4)]
        junk = sb(ph, nc, "junk", [128, D], BF16)
        xn = [sb(ph, nc, f"xn{i}", [128, D], BF16) for i in range(4)]
        ss = sb(ph, nc, "ss", [128, NT], F32)
        ms = sb(ph, nc, "ms", [128, NT], F32)
        rstd = sb(ph, nc, "rstd", [128, NT], F32)
        hT = [sb(ph, nc, f"hT{i}", [128, 8, 512], BF16) for i in range(2)]
        tp = [ps(ph, nc, f"tp{i}", [128, 8, 128], BF16) for i in range(2)]
        acc = [ps(ph, nc, f"acc{i}", [128, 512], F32) for i in range(5)]
        ssb = [ps(ph, nc, f"ssb{i}", [128, 512], F32) for i in range(1)]
        st_f = [sb(ph, nc, f"st_f{i}", [128, 512], F32) for i in range(2)]
        st_b = [sb(ph, nc, f"st_b{i}", [128, 512], BF16) for i in range(4)]
        z2 = [sb(ph, nc, f"z2{i}", [128, 512], F32) for i in range(2)]
        zu = [sb(ph, nc, f"zu{i}", [128, 512], F32) for i in range(2)]
        zt = [sb(ph, nc, f"zt{i}", [128, 512], F32) for i in range(2)]
        sq = [sb(ph, nc, f"sq{i}", [128, 512], BF16) for i in range(2)]
        msq = [sb(ph, nc, f"msq{i}", [128, 512], F32) for i in range(2)]
        rsq = [sb(ph, nc, f"rsq{i}", [128, 512], F32) for i in range(2)]
        gq8 = sb(ph, nc, "gq8", [128, 1], F32)
        sc_t = sb(ph, nc, "sc_t", [128, 8], F32)
        sc_f = sb(ph, nc, "sc_f", [128, 8], F32)
        sc_b = sb(ph, nc, "sc_b", [128, 8], BF16)
        wm = [sb(ph, nc, f"wm{i}", [128, 8, 1024], BF16) for i in range(2)]
        diag = [sb(ph, nc, f"diag{i}", [128, 128], F32) for i in range(2)]
        tk.op("dve", lambda v: v.tensor_scalar(out=gq8[:], in0=smc("gq"), scalar1=0.125, scalar2=None, op0=ALU.mult),
              r=["smalls"], w=["gq8"])
        ohrev = sb(ph, nc, "ohrev_sb", [32, 511], F32)
        rbk = sb(ph, nc, "rbk", [32, 4, 128], F32)
        fsb = sb(ph, nc, "fsb", [128, 4, 511], F32)
        tk.dma("sp", ohrev[:], cst["ohrev"], w=["ohrev"], stream="c5")
        tk.dma("sp", rbk[:], rbk_d, w=["rbk"], stream="c6")
        for h in range(4):
            fp = acc[h % 2][:, 0:511]
            tk.op("pe", lambda pe, h=h, fp=fp: pe.matmul(fp, lhsT=rbk[:, h, :], rhs=ohrev[:], start=True, stop=True),
                  r=["rbk", "ohrev"], w=[("acc", h % 2)])
            tk.op("dve", lambda v, h=h, fp=fp: v.tensor_copy(fsb[:, h, :], fp), r=[("acc", h % 2)], w=[("fsb", h)])
        tk.dma("sp", f_d.rearrange("h p n -> p h n"), fsb[:], r=[("fsb", h) for h in range(4)], w=["f_d"], stream="c7")
        wm_v = wmod_d.rearrange("(kc p) n -> p kc n", p=128)

        def load_wm(j):
            tk.dma("pool", wm[j % 2][:], wm_v[:, :, j * 1024:(j + 1) * 1024], w=[("wm", j % 2)], stream=f"wm{j % 2}")

        load_wm(0)
        load_wm(1)
        win_v = win_d.rearrange("(kc p) n -> p kc n", p=128)
        for kc in range(8):
            tk.dma("pool", win_b[:, kc, :], win_v[:, kc, :], w=[("win_b", kc)], stream=f"win{kc}")
        tk.op("act", lambda a: a.activation(out=sc_t[:], in_=smc("c"), func=AF.Tanh, scale=0.5),
              r=["smalls"], w=["sc_t"])
        tk.op("dve", lambda v: v.tensor_scalar(out=sc_f[:], in0=sc_t[:], scalar1=0.5, scalar2=0.5,
                                               op0=ALU.mult, op1=ALU.add), r=["sc_t"], w=["sc_f"])
        tk.op("dve", lambda v: v.tensor_tensor(out=sc_b[:], in0=sc_f[:], in1=smc("c"), op=ALU.mult),
              r=["sc_f", "smalls"], w=["sc_b"])

        def mod_mm(bank, j):
            for o8 in range(8):
                o = j * 8 + o8
                for kc in range(8):
                    tk.op("pe", lambda t, o=o, o8=o8, kc=kc: t.matmul(
                        bank[:, o:o + 1], lhsT=wm[j % 2][:, kc, o8 * 128:(o8 + 1) * 128], rhs=sc_b[:, kc:kc + 1],
                        start=(kc == 0), stop=(kc == 7)),
                        r=[("wm", j % 2), "sc_b"], w=[("acc", 3)] if bank is acc[3] else [], inc=(kc == 7 and o8 == 7))

        mod_mm(acc[3], 0)
        mod_mm(acc[3], 1)
        tk.op("dve", lambda v: v.tensor_tensor(out=modT[:, 0:16], in0=acc[3][:, 0:16], in1=smc("bmod", 0, 16),
                                               op=ALU.add), r=[("acc", 3), "smalls"], w=["modT"])
        tk.op("dve", lambda v: v.scalar_tensor_tensor(out=A1[:], in0=modT[:, 8:16], scalar=1.0, in1=smc("g1"),
                                                      op0=ALU.add, op1=ALU.mult), r=["modT", "smalls"], w=["A1"])
        load_wm(2)
        load_wm(3)

        def mk_mod(j0):
            st = {}

            def s0():
                a = cnt["acc"] % 5
                cnt["acc"] += 1
                st["a"] = a
                for j in (j0, j0 + 1):
                    for o8 in range(8):
                        o = j * 8 + o8
                        for kc in range(8):
                            tk.op("pe", lambda t, o=o, o8=o8, kc=kc, j=j: t.matmul(
                                acc[a][:, o:o + 1], lhsT=wm[j % 2][:, kc, o8 * 128:(o8 + 1) * 128],
                                rhs=sc_b[:, kc:kc + 1], start=(kc == 0), stop=(kc == 7)),
                                r=[("wm", j % 2), "sc_b"], w=[("acc", a)], inc=(kc == 7 and o8 == 7))
                if j0 + 3 < 6:
                    load_wm(j0 + 2)
                    load_wm(j0 + 3)

            def s1():
                a = st["a"]
                c0, c1 = j0 * 8, j0 * 8 + 16
                tk.op("dve", lambda v: v.tensor_tensor(out=modT[:, c0:c1], in0=acc[a][:, c0:c1],
                                                       in1=smc("bmod", c0, c1), op=ALU.add),
                      r=[("acc", a), "smalls"], w=["modT"])
                if j0 == 4:
                    tk.op("dve", lambda v: v.scalar_tensor_tensor(out=A2[:], in0=modT[:, 32:40], scalar=1.0,
                                                                  in1=smc("g2"), op0=ALU.add, op1=ALU.mult),
                          r=["modT", "smalls"], w=["A2"])

            return (s0, s1, lambda: None)

        def mk_bc(gi, half):
            srct, col0, dst = ((modT, 16, g1bc), (modT, 40, g2bc), (A2, 0, A2bc), (modT, 24, B2bc))[gi]
            st = {}

            def s0():
                a = cnt["acc"] % 5
                cnt["acc"] += 1
                st["a"] = a
                for k4 in range(4):
                    kc = half * 4 + k4
                    dslot = k4 % 2
                    tk.op("dve", lambda v, kc=kc, dslot=dslot: v.tensor_scalar(
                        out=diag[dslot][:], in0=ident_f[:], scalar1=srct[:, col0 + kc:col0 + kc + 1], scalar2=None,
                        op0=ALU.mult), r=["modT", "A2", "ident_f"], w=[("diag", dslot)])
                    tk.op("pe", lambda t, k4=k4, dslot=dslot: t.matmul(
                        acc[a][:, k4 * 128:(k4 + 1) * 128], lhsT=ones_f[:], rhs=diag[dslot][:], start=True, stop=True),
                        r=["ones_f", ("diag", dslot)], w=[("acc", a)])

            def s1():
                a = st["a"]
                tk.op("act", lambda e: e.copy(out=dst[:, half * 512:(half + 1) * 512], in_=acc[a][:]),
                      r=[("acc", a)], w=[("gbc", gi)])

            return (s0, s1, lambda: None)


        cnt = {"st_b": 0, "st_f": 0, "z": 0, "qk": 0, "acc": 0}

        def load_x(t):
            tk.dma("sp", xt[t % 4][:], x_d[t * 128:(t + 1) * 128, :], w=[("xt", t % 4)], stream=f"x{t % 4}")

        def front_a(t):
            s4, s2 = t % 4, t % 2
            if t + 2 < NT:
                load_x(t + 2)
            tk.op("act", lambda a: a.activation(out=junk[:], in_=xt[s4][:], func=AF.Square,
                                                accum_out=ss[:, t:t + 1]), r=[("xt", s4)], w=["junk", ("ss", t)])
            tk.op("act", lambda a: a.activation(out=ms[:, t:t + 1], in_=ss[:, t:t + 1], func=AF.Ln, scale=1.0 / D,
                                                bias=epsc[:, 0:1]), r=[("ss", t), "epsc"], w=[("ms", t)])
            tk.op("act", lambda a: a.activation(out=rstd[:, t:t + 1], in_=ms[:, t:t + 1], func=AF.Exp, scale=-0.5),
                  r=[("ms", t)], w=[("rstd", t)])
            tk.op("act", lambda a: a.activation(out=xn[s4][:], in_=xt[s4][:], func=AF.Copy,
                                                scale=rstd[:, t:t + 1]), r=[("xt", s4), ("rstd", t)], w=[("xn", s4)])

        def front_b(t):
            s2 = t % 2
            s4 = t % 4
            g, tl = t // 4, t % 4
            for kc in range(8):
                tk.op("pe", lambda pe, kc=kc: pe.transpose(tp[s2][:, kc, :], xn[s4][:, kc * 128:(kc + 1) * 128],
                                                           ident_b[:]),
                      r=[("xn", s4), "ident_b"], w=[("tp", s2)], inc=(kc == 7))
            for kc in range(8):
                tk.op("dve", lambda v, kc=kc: v.tensor_scalar(
                    out=hT[g % 2][:, kc, tl * 128:(tl + 1) * 128], in0=tp[s2][:, kc, :],
                    scalar1=A1[:, kc:kc + 1], scalar2=modT[:, kc:kc + 1], op0=ALU.mult, op1=ALU.add),
                    r=[("tp", s2), "A1", "modT"], w=[("hT", g % 2, tl)])

        def mk_fm(g, oc):
            st = {}
            tsl = slice(g * 512, (g + 1) * 512)
            hk = [("hT", g % 2, tl) for tl in range(4)]

            def s0():
                a = cnt["acc"] % 5
                cnt["acc"] += 1
                st["a"] = a
                for kc in range(8):
                    tk.op("pe", lambda pe, kc=kc: pe.matmul(acc[a][:], lhsT=win_b[:, kc, oc * 128:(oc + 1) * 128],
                                                            rhs=hT[g % 2][:, kc, :], start=(kc == 0), stop=(kc == 7)),
                          r=hk + [("win_b", kc)], w=[("acc", a)], inc=(kc == 7))
                if 4 <= oc < 8:
                    i = cnt["z"] % 2
                    cnt["z"] += 1
                    st["i"] = i
                    tk.op("act", lambda e: e.activation(out=z2[i][:], in_=acc[a][:], func=AF.Square),
                          r=[("acc", a)], w=[("z2", i)])
                    tk.op("dve", lambda v: v.tensor_scalar(out=z2[i][:], in0=z2[i][:], scalar1=C1 * 0.044715,
                                                           scalar2=C1, op0=ALU.mult, op1=ALU.add),
                          r=[("z2", i)], w=[("z2", i)])
                elif oc >= 8:
                    i = cnt["qk"] % 2
                    cnt["qk"] += 1
                    st["i"] = i
                    tk.op("act", lambda e: e.activation(out=sq[i][:], in_=acc[a][:], func=AF.Square),
                          r=[("acc", a)], w=[("sq", i)])

            def s1():
                a = st["a"]
                if oc < 4:
                    i = cnt["st_f"] % 2
                    cnt["st_f"] += 1
                    tk.op("dve", lambda v: v.tensor_copy(st_f[i][:], acc[a][:]), r=[("acc", a)], w=[("st_f", i)])
                    tk.dma("sp", xl_d[oc, :, tsl], st_f[i][:], r=[("st_f", i)], w=[("xl_d", oc, g)], stream=f"sf{i}")
                elif oc < 8:
                    i = st["i"]
                    tk.op("dve", lambda v: v.tensor_tensor(out=zu[i][:], in0=z2[i][:], in1=acc[a][:], op=ALU.mult),
                          r=[("z2", i), ("acc", a)], w=[("zu", i)])
                    tk.op("act", lambda e: e.activation(out=zt[i][:], in_=zu[i][:], func=AF.Tanh),
                          r=[("zu", i)], w=[("zt", i)])
                else:
                    i = st["i"]
                    tk.op("pe", lambda pe: pe.matmul(ssb[0][:], lhsT=b64_b[:], rhs=sq[i][:], start=True, stop=True),
                          r=["b64_b", ("sq", i)], w=[("ssb", 0)])
                    tk.op("act", lambda e: e.activation(out=msq[i][:], in_=ssb[0][:], func=AF.Ln, scale=1.0 / 64,
                                                        bias=epsc[:, 0:1]), r=[("ssb", 0), "epsc"], w=[("msq", i)])
                    tk.op("act", lambda e: e.activation(out=rsq[i][:], in_=msq[i][:], func=AF.Exp, scale=-0.5),
                          r=[("msq", i)], w=[("rsq", i)])

            def s2():
                a = st["a"]
                if oc < 4:
                    return
                j = cnt["st_b"] % 4
                cnt["st_b"] += 1
                i = st["i"]
                if oc < 8:
                    tk.op("dve", lambda v: v.scalar_tensor_tensor(out=st_b[j][:], in0=zt[i][:], scalar=1.0,
                                                                  in1=acc[a][:], op0=ALU.add, op1=ALU.mult),
                          r=[("zt", i), ("acc", a)], w=[("st_b", j)])
                    tk.dma("sp", gz_d[oc - 4, :, tsl], st_b[j][:], r=[("st_b", j)], w=[("gz_d", oc - 4, g)],
                           stream=f"sb{j}")
                else:
                    isq = oc < 12
                    gcol = gq8[:, 0:1] if isq else smc("gk")
                    tk.op("dve", lambda v: v.scalar_tensor_tensor(out=st_b[j][:], in0=acc[a][:], scalar=gcol,
                                                                  in1=rsq[i][:], op0=ALU.mult, op1=ALU.mult),
                          r=[("acc", a), ("rsq", i), "gq8", "smalls"], w=[("st_b", j)])
                    dst = qT_d if isq else kT_d
                    hd = (oc - 8) % 4
                    tk.dma("sp", dst[hd, :, tsl], st_b[j][:], r=[("st_b", j)], w=[("qk_d", oc, g)], stream=f"sb{j}")

            return (s0, s1, s2)

        def mk_v(g, tl):
            st = {}
            t = g * 4 + tl

            def s0():
                a = cnt["acc"] % 5
                cnt["acc"] += 1
                st["a"] = a
                for kc in range(8):
                    tk.op("pe", lambda pe, kc=kc: pe.matmul(acc[a][:], lhsT=hT[g % 2][:, kc, tl * 128:(tl + 1) * 128],
                                                            rhs=win_b[:, kc, 2048:2560], start=(kc == 0),
                                                            stop=(kc == 7)),
                          r=[("hT", g % 2, tl), ("win_b", kc)], w=[("acc", a)], inc=(kc == 7))

            def s1():
                a = st["a"]
                j = cnt["st_b"] % 4
                cnt["st_b"] += 1
                tk.op("dve", lambda v: v.tensor_copy(st_b[j][:], acc[a][:]), r=[("acc", a)], w=[("st_b", j)])
                tk.dma("sp", v_d[t * 128:(t + 1) * 128, :], st_b[j][:], r=[("st_b", j)], w=[("v_d", t)],
                       stream=f"sb{j}")

            return (s0, s1, lambda: None)

        load_x(0)
        load_x(1)
        front_a(0)
        front_a(1)
        front_b(0)
        front_a(2)
        front_b(1)
        front_a(3)
        front_b(2)
        front_b(3)
        chunks = []
        for g in range(NG):
            units = ([("fm", oc) for oc in range(4, 8)] + [("fm", oc) for oc in range(8, 16)]
                     + [("fm", oc) for oc in range(4)] + [("v", tl) for tl in range(4)])
            for ui, (kind, idx) in enumerate(units):
                chunks.append((mk_fm(g, idx) if kind == "fm" else mk_v(g, idx), g, ui))
            if g == 1:
                chunks.append((mk_mod(2), None, None))
            if g == 3:
                chunks.append((mk_mod(4), None, None))
            if g in (4, 5, 6, 7):
                gi = g - 4
                order = (2, 3, 0, 1)[gi] if False else gi
                chunks.append((mk_bc(order, 0), None, None))
                chunks.append((mk_bc(order, 1), None, None))
        for slot in range(len(chunks) + 2):
            if slot < len(chunks):
                chunks[slot][0][0]()
            if 0 <= slot - 1 < len(chunks):
                chunks[slot - 1][0][1]()
            if 0 <= slot - 2 < len(chunks):
                chunks[slot - 2][0][2]()
            if slot < len(chunks):
                _, g, ui = chunks[slot]
                if g is not None and g + 1 < NG:
                    if ui in (4, 6, 8, 10):
                        front_a((g + 1) * 4 + (ui - 4) // 2)
                    if ui in (8, 10, 12, 14):
                        front_b((g + 1) * 4 + (ui - 8) // 2)
        tk.barrier()
    if stop_after == "p1":
        return finish(B, out_d, [])

    with ExitStack() as ph:
        wbd = sb(ph, nc, "wbd", [128, 16, 128], BF16)
        lt = sb(ph, nc, "lt", [128, 8], F32)
        cl = sb(ph, nc, "cl", [128, 8], F32)
        hcl = sb(ph, nc, "hcl", [128, 8], F32)
        hba = sb(ph, nc, "hba", [128, 8], F32)
        hbx = sb(ph, nc, "hbx", [128, 8], F32)
        onec = sb(ph, nc, "onec", [128, 1], F32)
        xlp = sb(ph, nc, "xlp", [128, S + 4], F32)
        xc = [sb(ph, nc, f"xc{i}", [128, S], F32) for i in range(2)]
        xcb = sb(ph, nc, "xcb", [128, S], BF16)
        av = [sb(ph, nc, f"av{i}", [128, S], F32) for i in range(2)]
        a2v = [sb(ph, nc, f"a2v{i}", [128, S], F32) for i in range(2)]
        thxv = sb(ph, nc, "thxv", [128, S], F32)
        hf = sb(ph, nc, "hf", [128, S], F32)
        hb = sb(ph, nc, "hb", [128, S], F32)
        gzs = sb(ph, nc, "gzs", [128, S], BF16)
        th = [sb(ph, nc, f"th{i}", [128, 512], F32) for i in range(2)]
        pA = [ps(ph, nc, f"pA{i}", [128, 512], F32) for i in range(2)]
        pX = [ps(ph, nc, f"pX{i}", [128, 512], F32) for i in range(2)]
        tk.op("dve", lambda p: p.memset(wbd[:], 0.0), w=["wbd"])
        tk.op("dve", lambda p: p.memset(onec[:], 1.0), w=["onec"])
        tk.op("dve", lambda p: p.memset(xlp[:, 0:2], 0.0), w=["xlp_pad"])
        tk.op("dve", lambda p: p.memset(xlp[:, S + 2:S + 4], 0.0), w=["xlp_pad"])
        for gt, wsrc_ in enumerate((lwa_d, lwx_d)):
            for dr in range(2):
                for c in range(4):
                    for blk in range(2):
                        tk.dma("pool", wbd[blk * 64:(blk + 1) * 64, gt * 8 + dr * 4 + c, blk * 64:(blk + 1) * 64],
                               wsrc_[dr, 2 * c + blk], r=["wbd"], w=[("wbdl", gt, dr, c, blk)], stream="wbd")
        tk.op("act", lambda a: a.activation(out=lt[:], in_=smc("lam"), func=AF.Exp, scale=-1.0), r=["smalls"], w=["lt"])
        tk.op("act", lambda a: a.activation(out=lt[:], in_=lt[:], func=AF.Ln, bias=onec[:, 0:1]), r=["lt", "onec"],
              w=["lt"])
        tk.op("dve", lambda v: v.tensor_scalar(out=cl[:], in0=lt[:], scalar1=-8.0, scalar2=None, op0=ALU.mult),
              r=["lt"], w=["cl"])
        tk.op("dve", lambda v: v.tensor_scalar(out=hcl[:], in0=lt[:], scalar1=-4.0, scalar2=None, op0=ALU.mult),
              r=["lt"], w=["hcl"])
        tk.op("dve", lambda v: v.tensor_scalar(out=hba[:], in0=smc("ba"), scalar1=0.5, scalar2=None, op0=ALU.mult),
              r=["smalls"], w=["hba"])
        tk.op("dve", lambda v: v.tensor_scalar(out=hbx[:], in0=smc("bx"), scalar1=0.5, scalar2=None, op0=ALU.mult),
              r=["smalls"], w=["hbx"])
        wbd_keys = [("wbdl", gt, dr, c, blk) for gt in range(2) for dr in range(2) for c in range(4) for blk in range(2)]
        un = [0]

        def load_chunk(c):
            tk.dma("sp", xlp[:, 2:S + 2], xl_d[c], w=["xlp"], stream="xlp")

        def conv(c):
            xcc = xc[c % 2]
            cw = lambda j: smc("convw", c * 4 + j)
            tk.op("dve", lambda v: v.tensor_scalar(out=xcc[:], in0=xlp[:, 0:S], scalar1=cw(0), scalar2=smc("convb", c),
                                                   op0=ALU.mult, op1=ALU.add),
                  r=["xlp", "xlp_pad", "smalls"], w=[("xc", c % 2)])
            for j in range(1, 4):
                tk.op("dve", lambda v, j=j: v.scalar_tensor_tensor(out=xcc[:], in0=xlp[:, j:j + S], scalar=cw(j),
                                                                   in1=xcc[:], op0=ALU.mult, op1=ALU.add),
                      r=["xlp", "xlp_pad", "smalls", ("xc", c % 2)], w=[("xc", c % 2)])
            if c + 1 < 4:
                load_chunk(c + 1)

        def act_part(c, dr):
            col = dr * 4 + c
            for g in range(NG):
                i = un[0] % 2
                un[0] += 1
                ts_ = slice(g * 512, (g + 1) * 512)
                tk.op("pe", lambda pe: pe.matmul(pA[i][:], lhsT=wbd[:, 0 * 8 + col, :], rhs=xcb[:, ts_],
                                                 start=True, stop=True), r=["xcb", "wbd"] + wbd_keys, w=[("pA", i)])
                tk.op("act", lambda a: a.activation(out=th[i][:], in_=pA[i][:], func=AF.Tanh, scale=0.5,
                                                    bias=hba[:, col:col + 1]), r=[("pA", i), "hba"], w=[("th", i)])
                tk.op("act", lambda a: a.activation(out=av[dr][:, ts_], in_=th[i][:], func=AF.Exp,
                                                    scale=hcl[:, col:col + 1], bias=hcl[:, col:col + 1]),
                      r=[("th", i), "hcl"], w=[("av", dr)])
                tk.op("act", lambda a: a.activation(out=a2v[dr][:, ts_], in_=th[i][:], func=AF.Exp,
                                                    scale=cl[:, col:col + 1], bias=cl[:, col:col + 1]),
                      r=[("th", i), "cl"], w=[("a2v", dr)])
            for g in range(NG):
                i = un[0] % 2
                un[0] += 1
                ts_ = slice(g * 512, (g + 1) * 512)
                tk.op("pe", lambda pe: pe.matmul(pX[i][:], lhsT=wbd[:, 1 * 8 + col, :], rhs=xcb[:, ts_],
                                                 start=True, stop=True), r=["xcb", "wbd"] + wbd_keys, w=[("pX", i)])
                tk.op("act", lambda a: a.activation(out=thxv[:, ts_], in_=pX[i][:], func=AF.Tanh, scale=0.5,
                                                    bias=hbx[:, col:col + 1]), r=[("pX", i), "hbx"], w=["thxv"])
            tk.op("act", lambda a: a.activation(out=a2v[dr][:], in_=a2v[dr][:], func=AF.Ln, scale=-1.0,
                                                bias=onec[:, 0:1]), r=[("a2v", dr), "onec"], w=[("a2v", dr)])
            tk.op("act", lambda a: a.activation(out=a2v[dr][:], in_=a2v[dr][:], func=AF.Exp, scale=0.5),
                  r=[("a2v", dr)], w=[("a2v", dr)])

        def dve_part(c, dr):
            xcc = xc[c % 2]
            tk.op("dve", lambda v: v.scalar_tensor_tensor(out=thxv[:], in0=thxv[:], scalar=1.0, in1=xcc[:],
                                                          op0=ALU.add, op1=ALU.mult),
                  r=["thxv", ("xc", c % 2)], w=["thxv"])
            tk.op("dve", lambda v: v.scalar_tensor_tensor(out=a2v[dr][:], in0=thxv[:], scalar=0.5, in1=a2v[dr][:],
                                                          op0=ALU.mult, op1=ALU.mult),
                  r=["thxv", ("a2v", dr)], w=[("a2v", dr)])
            if dr == 0:
                tk.op("dve", lambda v: v.tensor_tensor_scan(out=hf[:], data0=av[0][:], data1=a2v[0][:], initial=0.0,
                                                            op0=ALU.mult, op1=ALU.add),
                      r=[("av", 0), ("a2v", 0)], w=["hf"])
            else:
                tk.op("dve", lambda v: v.tensor_tensor_scan(out=hb[:, ::-1], data0=av[1][:, ::-1],
                                                            data1=a2v[1][:, ::-1], initial=0.0, op0=ALU.mult,
                                                            op1=ALU.add), r=[("av", 1), ("a2v", 1)], w=["hb"])

        def finish_chunk(c):
            tk.op("dve", lambda v: v.tensor_tensor(out=hf[:], in0=hf[:], in1=hb[:], op=ALU.add),
                  r=["hf", "hb"], w=["hf"])
            tk.op("dve", lambda v: v.scalar_tensor_tensor(out=gzs[:], in0=hf[:], scalar=0.5, in1=gzs[:],
                                                          op0=ALU.mult, op1=ALU.mult), r=["hf", "gzs"], w=["gzs"])
            tk.dma("sp", yT_d[c], gzs[:], r=["gzs"], w=[("yT_d", c)], stream="ys")

        load_chunk(0)
        conv(0)
        for c in range(4):
            tk.dma("sp", gzs[:], gz_d[c], w=["gzs"], stream="gzs")
            tk.op("act", lambda a: a.copy(out=xcb[:], in_=xc[c % 2][:]), r=[("xc", c % 2)], w=["xcb"])
            act_part(c, 0)
            dve_part(c, 0)
            act_part(c, 1)
            if c + 1 < 4:
                conv(c + 1)
            dve_part(c, 1)
            finish_chunk(c)
        tk.barrier()
    if stop_after == "p2":
        return finish(B, out_d, [])

    with ExitStack() as ph:
        Tt = sb(ph, nc, "Tt", [128, 12, 128], F32)
        Dh = sb(ph, nc, "Dh", [128, 24, 512], BF16)
        Dl = sb(ph, nc, "Dl", [128, 24, 512], BF16)
        ncf = sb(ph, nc, "ncf", [128, 8], F32)
        lpr = sb(ph, nc, "lpr", [128, 128], F32)
        lsum = sb(ph, nc, "lsum", [128, 2], F32)
        lexp = sb(ph, nc, "lexp", [128, 2], F32)
        nlam = sb(ph, nc, "nlam", [128, 1], F32)
        gos = sb(ph, nc, "gos", [128, 128], F32)
        junk3 = sb(ph, nc, "junk3", [128, 128], F32)
        qh = [sb(ph, nc, f"qh{i}", [128, S], BF16) for i in range(2)]
        kh = [sb(ph, nc, f"kh{i}", [128, S], BF16) for i in range(2)]
        vh = [sb(ph, nc, f"vh{i}", [128, NT, 129], BF16) for i in range(2)]
        PT = [sb(ph, nc, f"PT{i}", [128, 1024], BF16) for i in range(3)]
        ocp = sb(ph, nc, "ocp", [128, 3, 3, 160], F32)
        rz = sb(ph, nc, "rz", [128, 9], F32)
        o0 = [sb(ph, nc, f"o0{i}", [128, 128], F32) for i in range(2)]
        oo4 = sb(ph, nc, "oo4", [128, 4, 128], F32)
        yb4 = sb(ph, nc, "yb4", [128, 4, 128], BF16)
        oss = sb(ph, nc, "oss", [128, 4], F32)
        oms = sb(ph, nc, "oms", [128, 4], F32)
        orstd = sb(ph, nc, "orstd", [128, 4], F32)
        yb = [sb(ph, nc, f"yb{i}", [128, 128], BF16) for i in range(2)]
        yst = [sb(ph, nc, f"yst{i}", [128, 512], BF16) for i in range(2)]
        sps = [ps(ph, nc, f"sps{i}", [128, 1024], F32) for i in range(2)]
        oacc = [ps(ph, nc, f"oacc{i}", [128, 512], F32) for i in range(3)]
        tpo = ps(ph, nc, "tpo", [128, 8, 128], BF16)

        def load_head(h):
            i = h % 2
            tk.dma("sp", qh[i][:], qT_d[h], w=[("qh", i)], stream=f"qh{i}")
            tk.dma("sp", kh[i][:], kT_d[h], w=[("kh", i)], stream=f"kh{i}")
            tk.dma("sp", vh[i][:, :, 0:128], v_d[:, h * 128:(h + 1) * 128].rearrange("(t p) d -> p t d", p=128),
                   r=[("vh1", i)], w=[("vh", i)], stream=f"vh{i}")

        for i in range(2):
            tk.op("dve", lambda p, i=i: p.memset(vh[i][:, :, 128:129], 1.0), w=[("vh1", i)])
        tk.op("dve", lambda p: p.memset(ocp[:], 1.0), w=[("ocp", 0), ("ocp", 1), ("ocp", 2)])
        load_head(0)
        for h in range(4):
            for dl in range(3):
                src = bass.AP(tensor=f_d.tensor, offset=h * 128 * 511 + 255 - 128 * (dl - 1), ap=[[510, 128], [1, 128]])
                tk.dma("sp", Tt[:, h * 3 + dl, :], src, w=[("Tt", h, dl)], stream=f"tt{h * 3 + dl}")
        tk.op("dve", lambda v: v.tensor_scalar(out=ncf[:, 0:4], in0=smc("rb", 60, 64), scalar1=-1.0, scalar2=None,
                                               op0=ALU.mult), r=["smalls"], w=["ncf"])
        tk.op("dve", lambda v: v.tensor_scalar(out=ncf[:, 4:8], in0=smc("rb", 124, 128), scalar1=-1.0, scalar2=None,
                                               op0=ALU.mult), r=["smalls", "ncf"], w=["ncf"])
        def build_bias_blocks(h):
            for dk in range(-1, 5):
                ty = 0 if dk <= 1 else 1
                slot = h * 6 + dk + 1
                nccol = ncf[:, ty * 4 + h:ty * 4 + h + 1]
                for ql in range(4):
                    dl = dk - ql
                    if -1 <= dl <= 1:
                        cs = slice(ql * 128, (ql + 1) * 128)
                        tk.op("act", lambda a, dl=dl, cs=cs: a.activation(
                            out=Dh[:, slot, cs], in_=Tt[:, h * 3 + dl + 1, :], func=AF.Identity, bias=nccol),
                            r=[("Tt", h, dl + 1), "ncf"], w=[("Dh", slot)])
                        tk.op("dve", lambda v, dl=dl, cs=cs: v.scalar_tensor_tensor(
                            out=Dl[:, slot, cs], in0=Tt[:, h * 3 + dl + 1, :], scalar=nccol, in1=Dh[:, slot, cs],
                            op0=ALU.add, op1=ALU.subtract),
                            r=[("Tt", h, dl + 1), "ncf", ("Dh", slot)], w=[("Dl", slot)])

        build_bias_blocks(0)
        lq = lambda i: smc("lqk", i * 64, (i + 1) * 64)
        tk.op("dve", lambda v: v.tensor_tensor(out=lpr[:, 0:64], in0=lq(0), in1=lq(1), op=ALU.mult),
              r=["smalls"], w=["lpr"])
        tk.op("dve", lambda v: v.tensor_tensor(out=lpr[:, 64:128], in0=lq(2), in1=lq(3), op=ALU.mult),
              r=["smalls", "lpr"], w=["lpr"])
        for i in range(2):
            tk.op("act", lambda a, i=i: a.activation(out=junk3[:, 0:64], in_=lpr[:, i * 64:(i + 1) * 64], func=AF.Copy,
                                                     accum_out=lsum[:, i:i + 1]), r=["lpr"], w=["junk3", ("lsum", i)])
        tk.op("act", lambda a: a.activation(out=lexp[:], in_=lsum[:], func=AF.Exp), r=[("lsum", 0), ("lsum", 1)],
              w=["lexp"])
        tk.op("dve", lambda v: v.scalar_tensor_tensor(out=nlam[:], in0=lexp[:, 1:2], scalar=-LAM_INIT, in1=lexp[:, 0:1],
                                                      op0=ALU.add, op1=ALU.subtract), r=["lexp"], w=["nlam"])
        tk.op("dve", lambda v: v.tensor_scalar(out=gos[:], in0=smc("go"), scalar1=1.0 - LAM_INIT, scalar2=None,
                                               op0=ALU.mult), r=["smalls"], w=["gos"])

        pairs = [(h, qg, kc) for h in range(4) for qg in range(NG) for kc in range(NT)]
        NP = len(pairs)

        def emit_qk(n):
            h, qg, kc = pairs[n]
            i = h % 2
            b = n % 2
            dk = kc - 4 * qg
            near = -1 <= dk <= 4
            for j in range(2):
                osl = sps[b][:, j * 512:(j + 1) * 512]
                tk.op("pe", lambda pe: pe.matmul(osl, lhsT=kh[i][j * 64:(j + 1) * 64, kc * 128:(kc + 1) * 128],
                                                 rhs=qh[i][j * 64:(j + 1) * 64, qg * 512:(qg + 1) * 512],
                                                 start=True, stop=not near),
                      r=[("kh", i), ("qh", i)], w=[("sps", b)], inc=(j == 1 and not near))
            if near:
                slot = h * 6 + dk + 1
                qls = [ql for ql in range(4) if -1 <= dk - ql <= 1]
                c0, c1 = qls[0] * 128, (qls[-1] + 1) * 128
                for j in range(2):
                    osl = sps[b][:, j * 512 + c0:j * 512 + c1]
                    tk.op("pe", lambda pe: pe.matmul(osl, lhsT=ident_b[:], rhs=Dh[:, slot, c0:c1], start=False,
                                                     stop=False), r=["ident_b", ("Dh", slot)], w=[("sps", b)], inc=False)
                    tk.op("pe", lambda pe: pe.matmul(osl, lhsT=ident_b[:], rhs=Dl[:, slot, c0:c1], start=False,
                                                     stop=True), r=["ident_b", ("Dl", slot)], w=[("sps", b)],
                          inc=(j == 1))

        def emit_exp(n):
            h, qg, kc = pairs[n]
            b = n % 2
            ty = 0 if kc <= 4 * qg + 1 else 1
            tk.op("act", lambda a: a.activation(out=PT[n % 3][:], in_=sps[b][:], func=AF.Exp,
                                                bias=smc("rb", (15 + 16 * ty) * 4 + h)),
                  r=[("sps", b), "smalls"], w=[("PT", n % 3)])

        def emit_pv(n):
            h, qg, kc = pairs[n]
            i = h % 2
            for j in range(2):
                for ql in range(4):
                    a_ = j * 4 + ql
                    bank, off = a_ // 3, (a_ % 3) * 160
                    tk.op("pe", lambda pe, ql=ql, bank=bank, off=off, j=j: pe.matmul(
                        oacc[bank][:, off:off + 129], lhsT=PT[n % 3][:, j * 512 + ql * 128:j * 512 + (ql + 1) * 128],
                        rhs=vh[i][:, kc, :], start=(kc == 0 and a_ % 3 == 0), stop=(kc == NT - 1),
                        skip_group_check=True),
                        r=[("PT", n % 3), ("vh", i), ("vh1", i)], w=[("oacc", bank)], inc=(j == 1 and ql == 3))
            if kc == NT - 1:
                combine(h, qg)

        pending = []

        def combine(h, qg):
            for bank in range(3):
                na = 3 if bank < 2 else 2
                tk.op("dve", lambda v, bank=bank, na=na: v.tensor_copy(
                    ocp[:, bank, 0:na, 0:129],
                    oacc[bank][:, 0:480].rearrange("p (a c) -> p a c", c=160)[:, 0:na, 0:129]),
                    r=[("oacc", bank)], w=[("ocp", bank)])
            ock = [("ocp", bk) for bk in range(3)]
            tk.op("dve", lambda v: v.reciprocal(out=rz[:].rearrange("p (a b) -> p a b", b=3), in_=ocp[:, :, :, 128]),
                  r=ock, w=["rz"])
            tk.op("dve", lambda v: v.tensor_scalar(out=rz[:, 4:8], in0=rz[:, 4:8], scalar1=nlam[:, 0:1], scalar2=None,
                                                   op0=ALU.mult), r=["rz", "nlam"], w=["rz"])
            for ql in range(4):
                a0, a1 = ql, 4 + ql
                tk.op("dve", lambda v: v.tensor_scalar(out=o0[0][:], in0=ocp[:, a0 // 3, a0 % 3, 0:128],
                                                       scalar1=rz[:, a0:a0 + 1], scalar2=None, op0=ALU.mult),
                      r=ock + ["rz"], w=[("o0", 0)])
                tk.op("dve", lambda v: v.scalar_tensor_tensor(out=oo4[:, ql, :], in0=ocp[:, a1 // 3, a1 % 3, 0:128],
                                                              scalar=rz[:, a1:a1 + 1], in1=o0[0][:],
                                                              op0=ALU.mult, op1=ALU.add),
                      r=ock + ["rz", ("o0", 0)], w=[("oo4", ql)])
                tk.op("dve", lambda v: v.scalar_tensor_tensor(out=junk3[:], in0=oo4[:, ql, :], scalar=1.0,
                                                              in1=oo4[:, ql, :], op0=ALU.mult, op1=ALU.mult,
                                                              accum_out=oss[:, ql:ql + 1]),
                      r=[("oo4", ql)], w=["junk3", ("oss", ql)])
            ossk = [("oss", q) for q in range(4)]
            tk.op("pool", lambda p: p.tensor_scalar(out=oms[:], in0=oss[:], scalar1=1.0 / 128, scalar2=EPS,
                                                    op0=ALU.mult, op1=ALU.add), r=ossk, w=["oms"])
            tk.op("pool", lambda p: p.tensor_tensor(out=orstd[:], in0=oms[:], in1=mhalf[:, 0:4], op=ALU.pow),
                  r=["oms", "mhalf"], w=["orstd"])
            for ql in range(4):
                tk.op("dve", lambda v: v.scalar_tensor_tensor(out=yb4[:, ql, :], in0=oo4[:, ql, :],
                                                              scalar=orstd[:, ql:ql + 1], in1=gos[:],
                                                              op0=ALU.mult, op1=ALU.mult),
                      r=[("oo4", ql), "orstd", "gos"], w=[("yb4", ql)])
            pending.append((h, qg))

        def combine_b(h, qg):
            for ql in range(4):
                tk.op("pe", lambda pe: pe.transpose(tpo[:, ql, :], yb4[:, ql, :], ident_b[:]),
                      r=[("yb4", ql), "ident_b"], w=["tpo"], inc=(ql == 3))
            ysl = (h * NG + qg) % 2
            tk.op("dve", lambda v: v.tensor_copy(yst[ysl][:], tpo[:, 0:4, :]), r=["tpo"], w=[("yst", ysl)])
            tk.dma("sp", yT_d[4 + h, :, qg * 512:(qg + 1) * 512], yst[ysl][:], r=[("yst", ysl)],
                   w=[("yT_d", 4 + h, qg)], stream=f"yst{ysl}")

        emit_qk(0)
        emit_exp(0)
        emit_qk(1)
        emit_exp(1)
        for n in range(NP):
            h, qg, kc = pairs[n]
            if qg == 0 and kc == 0 and h + 1 < 4:
                load_head(h + 1)
            if qg == 2 and kc == 16 and h + 1 < 4:
                build_bias_blocks(h + 1)
            if n + 2 < NP:
                emit_qk(n + 2)
                emit_exp(n + 2)
            emit_pv(n)
            if kc == 8 and pending:
                combine_b(*pending.pop(0))
        while pending:
            combine_b(*pending.pop(0))
        tk.barrier()
    if stop_after == "p3":
        return finish(B, out_d, [])

    moe = ExitStack()
    wbuf = [[sb(moe, nc, f"wexp{i}_{m}", [128, 8, D], BF16) for m in range(3)] for i in range(2)]
    wsrc = (w1_d, w3_d, w2_d)

    def load_expert(e):
        for m in range(3):
            v = wsrc[m][e].rearrange("(kc p) n -> p kc n", p=128)
            for hh in range(2):
                tk.dma("pool", wbuf[e % 2][m][:, hh * 4:(hh + 1) * 4, :], v[:, hh * 4:(hh + 1) * 4, :],
                       w=[("wexp", e % 2, m, hh)], stream=f"we{e % 2}{m}{hh}")

    with ExitStack() as ph:
        wo_b = sb(ph, nc, "wo_b", [128, 8, D], BF16)
        wr_b = sb(ph, nc, "wr_b", [128, 128], BF16)
        yTg = [sb(ph, nc, f"yTg{i}", [128, 8, 512], BF16) for i in range(2)]
        xt = [sb(ph, nc, f"xt4_{i}", [128, D], F32) for i in range(3)]
        tmp = [sb(ph, nc, f"tmp4_{i}", [128, D], F32) for i in range(2)]
        x1 = [sb(ph, nc, f"x1_{i}", [128, D], F32) for i in range(2)]
        h2f = [sb(ph, nc, f"h2f{i}", [128, D], F32) for i in range(2)]
        h2b = [sb(ph, nc, f"h2b{i}", [128, D], BF16) for i in range(2)]
        h2T = [sb(ph, nc, f"h2T{i}", [128, 8, 128], BF16) for i in range(2)]
        junk = sb(ph, nc, "junk4", [128, D], BF16)
        ss = sb(ph, nc, "ss4", [128, NT], F32)
        ms = sb(ph, nc, "ms4", [128, NT], F32)
        rstd = sb(ph, nc, "rstd4", [128, NT], F32)
        mx = sb(ph, nc, "mx4", [128, NT], F32)
        esum = sb(ph, nc, "esum4", [128, NT], F32)
        resum = sb(ph, nc, "resum4", [128, NT], F32)
        eaff = [sb(ph, nc, f"eaff{i}", [128, NE], F32) for i in range(2)]
        mix = [[ps(ph, nc, f"mix{i}_{hh}", [128, 512], F32) for hh in range(2)] for i in range(2)]
        tp = [ps(ph, nc, f"tp4_{i}", [128, 8, 128], BF16) for i in range(2)]
        lg = [ps(ph, nc, f"lg{i}", [128, 512], F32) for i in range(2)]
        wo_v = wout_d.rearrange("(kc p) n -> p kc n", p=128)
        for hh in range(2):
            tk.dma("pool", wo_b[:, hh * 4:(hh + 1) * 4, :], wo_v[:, hh * 4:(hh + 1) * 4, :], w=[("wo_b", hh)],
                   stream=f"wo{hh}")
        tk.op("dve", lambda v: v.tensor_copy(wr_b[:], smc("wr")), r=["smalls"], w=["wr_b"])
        for kc in range(8):
            tk.op("dve", lambda v, kc=kc: v.tensor_tensor(out=wo_b[:, kc, :], in0=wo_b[:, kc, :], in1=g1bc[:],
                                                          op=ALU.mult), r=[("wo_b", kc // 4), ("gbc", 0)],
                  w=[("wo_b", kc // 4)])
        load_expert(0)
        load_expert(1)

        def load_g(g):
            tk.dma("sp", yTg[g % 2][:], yT_d[:, :, g * 512:(g + 1) * 512].rearrange("k p n -> p k n"),
                   w=[("yTg", g % 2, kc) for kc in range(8)], stream=f"yg{g % 2}")

        def load_x4(t):
            tk.dma("sp", xt[t % 3][:], x_d[t * 128:(t + 1) * 128, :], w=[("xt4", t % 3)], stream=f"x4{t % 3}")

        load_g(0)
        load_x4(0)
        load_x4(1)

        def p4_s0(t):
            g, tl = t // 4, t % 4
            i2 = t % 2
            if tl == 0 and g + 1 < NG:
                load_g(g + 1)
            if t + 2 < NT:
                load_x4(t + 2)
            for hh in range(2):
                for kc in range(8):
                    tk.op("pe", lambda pe, hh=hh, kc=kc: pe.matmul(
                        mix[i2][hh][:], lhsT=yTg[g % 2][:, kc, tl * 128:(tl + 1) * 128],
                        rhs=wo_b[:, kc, hh * 512:(hh + 1) * 512], start=(kc == 0), stop=(kc == 7)),
                        r=[("yTg", g % 2, kc), ("wo_b", kc // 4)], w=[("mix", i2, hh)], inc=(kc == 7))
            for hh in range(2):
                cs = slice(hh * 512, (hh + 1) * 512)
                tk.op("dve", lambda v, hh=hh, cs=cs: v.tensor_tensor(out=x1[i2][:, cs], in0=mix[i2][hh][:],
                                                                     in1=xt[t % 3][:, cs], op=ALU.add),
                      r=[("mix", i2, hh), ("xt4", t % 3)], w=[("x1", i2)])
            tk.dma("sp", out_d[t * 128:(t + 1) * 128, :], x1[i2][:], r=[("x1", i2)], w=[("out_d", t)],
                   stream=f"ox{i2}")
            tk.op("act", lambda a: a.activation(out=junk[:], in_=x1[i2][:], func=AF.Square, accum_out=ss[:, t:t + 1]),
                  r=[("x1", i2)], w=["junk4", ("ss4", t)])
            tk.op("act", lambda a: a.activation(out=ms[:, t:t + 1], in_=ss[:, t:t + 1], func=AF.Ln, scale=1.0 / D,
                                                bias=epsc[:, 0:1]), r=[("ss4", t), "epsc"], w=[("ms4", t)])
            tk.op("act", lambda a: a.activation(out=rstd[:, t:t + 1], in_=ms[:, t:t + 1], func=AF.Exp, scale=-0.5),
                  r=[("ms4", t)], w=[("rstd4", t)])

        def p4_s1(t):
            i2 = t % 2
            tk.op("dve", lambda v: v.scalar_tensor_tensor(out=h2f[i2][:], in0=x1[i2][:], scalar=rstd[:, t:t + 1],
                                                          in1=A2bc[:], op0=ALU.mult, op1=ALU.mult),
                  r=[("x1", i2), ("rstd4", t), ("gbc", 2)], w=[("h2f", i2)])
            tk.op("dve", lambda p: p.tensor_tensor(out=h2b[i2][:], in0=h2f[i2][:], in1=B2bc[:], op=ALU.add),
                  r=[("h2f", i2), ("gbc", 3)], w=[("h2b", i2)])
            tk.dma("sp", h2_d[t * 128:(t + 1) * 128, :], h2b[i2][:], r=[("h2b", i2)], w=[("h2_d", t)],
                   stream=f"oh{i2}")

        def p4_s1b(t):
            i2 = t % 2
            for kc in range(8):
                tk.op("pe", lambda pe, kc=kc: pe.transpose(tp[i2][:, kc, :], h2b[i2][:, kc * 128:(kc + 1) * 128],
                                                           ident_b[:]),
                      r=[("h2b", i2), "ident_b"], w=[("tp4", i2)], inc=(kc == 7))
            tk.op("act", lambda a: a.copy(out=h2T[i2][:], in_=tp[i2][:]), r=[("tp4", i2)], w=[("h2T", i2)])

        def p4_s2(t):
            i2 = t % 2
            for kc in range(8):
                tk.op("pe", lambda pe, kc=kc: pe.matmul(lg[i2][:, 0:NE], lhsT=h2T[i2][:, kc, :],
                                                        rhs=wr_b[:, kc * NE:(kc + 1) * NE], start=(kc == 0),
                                                        stop=(kc == 7)),
                      r=[("h2T", i2), "wr_b"], w=[("lg", i2)], inc=(kc == 7))
            tk.op("dve", lambda v: v.reduce_max(out=mx[:, t:t + 1], in_=lg[i2][:, 0:NE], axis=AX.X),
                  r=[("lg", i2)], w=[("mx4", t)])
            tk.op("dve", lambda v: v.tensor_scalar(out=mx[:, t:t + 1], in0=mx[:, t:t + 1], scalar1=-1.0, scalar2=None,
                                                   op0=ALU.mult), r=[("mx4", t)], w=[("mx4", t)])
            tk.op("act", lambda a: a.activation(out=eaff[i2][:], in_=lg[i2][:, 0:NE], func=AF.Exp,
                                                bias=mx[:, t:t + 1], accum_out=esum[:, t:t + 1]),
                  r=[("lg", i2), ("mx4", t)], w=[("eaff", i2), ("esum4", t)])
            tk.op("dve", lambda v: v.reciprocal(out=resum[:, t:t + 1], in_=esum[:, t:t + 1]),
                  r=[("esum4", t)], w=[("resum4", t)])
            tk.op("dve", lambda v: v.tensor_scalar(out=affT[:, t, :], in0=eaff[i2][:], scalar1=resum[:, t:t + 1],
                                                   scalar2=None, op0=ALU.mult),
                  r=[("eaff", i2), ("resum4", t)], w=[("affT", t)])

        for slot in range(NT + 3):
            if slot < NT:
                p4_s0(slot)
            if 0 <= slot - 1 < NT:
                p4_s1(slot - 1)
            if 0 <= slot - 2 < NT:
                p4_s1b(slot - 2)
            if 0 <= slot - 3 < NT:
                p4_s2(slot - 3)
        tk.barrier()
    if stop_after == "p4":
        moe.close()
        return finish(B, out_d, [])

    idx_i = sb(moe, nc, "idx_i", [128, 4 * NE], I32)
    gsel = sb(moe, nc, "gsel", [128, 4 * NE], F32)
    L4 = sb(moe, nc, "L4", [128, 4, NT, NE], BF16)
    pm = sb(moe, nc, "pm", [128, NT, NE], F32)
    iota1 = sb(moe, nc, "iota1_sb", [128, 512], F32)
    oh = [sb(moe, nc, f"oh{i}", [128, 512], BF16) for i in range(8)]
    Rs = sb(moe, nc, "Rs", [128, 16], F32)
    idxf = sb(moe, nc, "idxf", [128, 4], F32)
    R_ps = ps(moe, nc, "R_ps", [128, 2, 256], F32)
    with ExitStack() as ph:
        affE = sb(ph, nc, "affE", [128, 512], F32)
        junkE = sb(ph, nc, "junkE", [128, 512], BF16)
        maskE = sb(ph, nc, "maskE", [128, 512], BF16)
        gsum = sb(ph, nc, "gsum_sb", [128, 128], F32)
        lo = sb(ph, nc, "lo", [128, 1], F32)
        mid = sb(ph, nc, "mid", [128, 1], F32)
        cntt = sb(ph, nc, "cntt", [128, 1], F32)
        ge = sb(ph, nc, "ge", [128, 1], F32)
        mask_tm = sb(ph, nc, "mask_tm", [128, NT, NE], BF16)
        ut_b = sb(ph, nc, "ut_b_sb", [128, 128], BF16)
        ones_b = sb(ph, nc, "ones_b_sb", [128, 128], BF16)
        totE = sb(ph, nc, "totE", [128, NE, NT], F32)
        flg = sb(ph, nc, "flg", [128, NE, NT], F32)
        cumE = sb(ph, nc, "cumE", [128, NE, NT], F32)
        p1 = sb(ph, nc, "p1", [128, NT, NE], F32)
        tpa = ps(ph, nc, "tpa", [128, 512], F32)
        cnt_ps = ps(ph, nc, "cnt_ps", [128, 512], F32)
        tpm = ps(ph, nc, "tpm", [128, NT, NE], BF16)
        pfx_ps = ps(ph, nc, "pfx_ps", [128, 512], F32)
        tot_ps = ps(ph, nc, "tot_ps", [128, 512], F32)
        tk.dma("sp", ut_b[:], cst["ut_b"], w=["ut_b"], stream="c0")
        tk.dma("sp", ones_b[:], cst["ones_b"], w=["ones_b"], stream="c1")
        tk.dma("sp", iota1[:], cst["iota1"], w=["iota1"], stream="c2")
        tk.dma("sp", L4[:, 0:2, :, :], cst["tokab"], w=[("L4", 0)], stream="c3")
        tk.dma("sp", gsum[:], cst["gsum"], w=["gsum"], stream="c4")
        tk.op("dve", lambda p: p.memset(flg[:], 1.0), w=["flg"])
        tk.op("dve", lambda p: p.memset(flg[:, :, 0:1], 0.0), r=["flg"], w=["flg"])
        tk.op("dve", lambda p: p.memset(lo[:], 0.0), w=["lo"])
        tk.op("act", lambda a: a.copy(out=L4[:, 2, :, :], in_=affT[:]), r=[("affT", t) for t in range(NT)],
              w=[("L4", 2)])
        tk.op("dve", lambda p: p.tensor_tensor(out=L4[:, 3, :, :], in0=affT[:], in1=L4[:, 2, :, :], op=ALU.subtract),
              r=[("affT", t) for t in range(NT)] + [("L4", 2)], w=[("L4", 3)])
        for q4 in range(4):
            tk.op("pe", lambda pe, q4=q4: pe.transpose(
                tpa[:, q4 * 128:(q4 + 1) * 128], affT[:, q4 * 8:(q4 + 1) * 8, :].rearrange("p t e -> p (t e)"),
                ident_f[:]), r=[("affT", t) for t in range(q4 * 8, q4 * 8 + 8)] + ["ident_f"], w=["tpa"])
        tk.op("act", lambda a: a.copy(out=affE[:], in_=tpa[:]), r=["tpa"], w=["affE"])
        for it in range(30):
            wdt = 2.0 ** -(it + 1)
            tk.op("dve", lambda v: v.tensor_scalar(out=mid[:], in0=lo[:], scalar1=wdt, scalar2=None, op0=ALU.add),
                  r=["lo"], w=["mid"])
            tk.op("dve", lambda v: v.tensor_scalar(out=junkE[:], in0=affE[:], scalar1=mid[:, 0:1], scalar2=None,
                                                   op0=ALU.is_gt, op1=ALU.add, accum_out=cntt[:, 0:1]),
                  r=["affE", "mid"], w=["junkE", "cntt"])
            tk.op("pe", lambda pe: pe.matmul(cnt_ps[:, 0:1], lhsT=gsum[:], rhs=cntt[:, 0:1], start=True, stop=True),
                  r=["gsum", "cntt"], w=["cnt_ps"])
            tk.op("dve", lambda v: v.tensor_scalar(out=ge[:], in0=cnt_ps[:, 0:1], scalar1=float(CAP), scalar2=None,
                                                   op0=ALU.is_ge), r=["cnt_ps"], w=["ge"])
            tk.op("dve", lambda v: v.scalar_tensor_tensor(out=lo[:], in0=mid[:], scalar=ge[:, 0:1], in1=lo[:],
                                                          op0=ALU.mult, op1=ALU.max), r=["mid", "ge", "lo"], w=["lo"])
        tk.op("dve", lambda v: v.tensor_scalar(out=maskE[:], in0=affE[:], scalar1=lo[:, 0:1], scalar2=None,
                                               op0=ALU.is_gt), r=["affE", "lo"], w=["maskE"])
        for q4 in range(4):
            tk.op("pe", lambda pe, q4=q4: pe.transpose(tpm[:, q4 * 8:(q4 + 1) * 8, :].rearrange("p t e -> p (t e)"),
                                                       maskE[:, q4 * 128:(q4 + 1) * 128], ident_b[:]),
                  r=["maskE", "ident_b"], w=["tpm"], inc=(q4 == 3))
        tk.op("act", lambda a: a.copy(out=mask_tm[:], in_=tpm[:]), r=["tpm"], w=["mask_tm"])
        mflat = mask_tm[:].rearrange("p t e -> p (t e)")
        tk.op("pe", lambda pe: pe.matmul(pfx_ps[:], lhsT=ut_b[:], rhs=mflat, start=True, stop=True),
              r=["ut_b", "mask_tm"], w=["pfx_ps"])
        tk.op("pe", lambda pe: pe.matmul(tot_ps[:], lhsT=ones_b[:], rhs=mflat, start=True, stop=True),
              r=["ones_b", "mask_tm"], w=["tot_ps"])
        tk.op("dve", lambda v: v.tensor_copy(totE[:].rearrange("p e t -> p t e"),
                                             tot_ps[:].rearrange("p (t e) -> p t e", e=NE)),
              r=["tot_ps"], w=["totE"])
        tk.op("dve", lambda v: v.tensor_tensor_scan(out=cumE[:].rearrange("p e t -> p (e t)"),
                                                    data0=flg[:].rearrange("p e t -> p (e t)"),
                                                    data1=totE[:].rearrange("p e t -> p (e t)"), initial=0.0,
                                                    op0=ALU.mult, op1=ALU.add), r=["flg", "totE"], w=["cumE"])
        tk.op("dve", lambda v: v.tensor_tensor(out=cumE[:], in0=cumE[:], in1=totE[:], op=ALU.subtract),
              r=["cumE", "totE"], w=["cumE"])
        tk.op("dve", lambda v: v.tensor_tensor(out=p1[:], in0=pfx_ps[:].rearrange("p (t e) -> p t e", e=NE),
                                               in1=cumE[:].rearrange("p e t -> p t e"), op=ALU.add),
              r=["pfx_ps", "cumE"], w=["p1"])
        tk.op("dve", lambda v: v.tensor_tensor(out=pm[:], in0=p1[:], in1=mask_tm[:], op=ALU.mult),
              r=["p1", "mask_tm"], w=["pm"])
        tk.barrier()

    ohc = [0]

    def build_idx_gen(e):
        rb_ = 0
        base = ohc[0]
        ohc[0] += NT

        def onehot(t):
            o = (base + t) % 8
            tk.op("dve", lambda v: v.tensor_scalar(out=oh[o][:], in0=iota1[:], scalar1=pm[:, t, e:e + 1],
                                                   scalar2=None, op0=ALU.is_equal),
                  r=["iota1", "pm"], w=[("oh", o)])

        LA = 5
        for t in range(LA):
            onehot(t)
        yield
        for t in range(NT):
            o = (base + t) % 8
            if t + LA < NT:
                onehot(t + LA)
            for j in range(4):
                tk.op("pe", lambda pe, t=t, o=o, j=j: pe.matmul(
                    R_ps[:, rb_, j * 4:(j + 1) * 4], lhsT=oh[o][:, j * 128:(j + 1) * 128], rhs=L4[:, :, t, e],
                    start=(t == 0 and j == 0), stop=(t == NT - 1), skip_group_check=True),
                    r=[("oh", o), ("L4", 0), ("L4", 2), ("L4", 3)], w=["R_ps"], inc=(j == 3))
            if t < NT - 1:
                yield
        tk.op("dve", lambda v: v.tensor_copy(Rs[:], R_ps[:, rb_, 0:16]), r=["R_ps"], w=["Rs"])
        Rv = Rs[:].rearrange("p (j c) -> p j c", c=4)
        tk.op("dve", lambda v: v.scalar_tensor_tensor(out=idxf[:], in0=Rv[:, :, 0], scalar=64.0, in1=Rv[:, :, 1],
                                                      op0=ALU.mult, op1=ALU.add), r=["Rs"], w=["idxf"])
        tk.op("dve", lambda v: v.tensor_copy(idx_i[:, e * 4:(e + 1) * 4], idxf[:]), r=["idxf"], w=[("idx", e)])
        tk.op("dve", lambda v: v.tensor_tensor(out=gsel[:, e * 4:(e + 1) * 4], in0=Rv[:, :, 2], in1=Rv[:, :, 3],
                                               op=ALU.add), r=["Rs"], w=[("gsel", e)])
        yield

    def build_idx(e):
        for _ in build_idx_gen(e):
            pass

    if stop_after == "p5":
        dbg_idx = B.dram_scratch("dbg_idx", [128, 4 * NE], I32)
        dbg_g = B.dram_scratch("dbg_g", [128, 4 * NE], F32)
        dbg_pm = B.dram_scratch("dbg_pm", [128, NT * NE], F32)
        for e in range(NE):
            build_idx(e)
        tk.dma("sp", dbg_idx, idx_i[:], r=[("idx", e) for e in range(NE)], w=["dbg_idx"], stream="c0")
        tk.dma("sp", dbg_g, gsel[:], r=[("gsel", e) for e in range(NE)], w=["dbg_g"], stream="c1")
        tk.dma("sp", dbg_pm, pm[:].rearrange("p t e -> p (t e)"), r=["pm"], w=["dbg_pm"], stream="c2")
        moe.close()
        return finish(B, out_d, [])

    with ExitStack() as ph:
        xe = [sb(ph, nc, f"xe{i}", [128, D], BF16) for i in range(8)]
        xeT = [sb(ph, nc, f"xeT{i}", [128, 8, 512], BF16) for i in range(2)]
        hT = sb(ph, nc, "hT6", [128, 8, 512], BF16)
        thh = [sb(ph, nc, f"thh{i}", [128, 512], F32) for i in range(2)]
        t1h = [sb(ph, nc, f"t1h{i}", [128, 512], F32) for i in range(2)]
        ysc = [sb(ph, nc, f"ysc{i}", [128, D], F32) for i in range(4)]
        tpx = ps(ph, nc, "tpx", [128, 2, 4, 128], BF16)
        pa = [ps(ph, nc, f"pa{i}", [128, 512], F32) for i in range(2)]
        pb = [ps(ph, nc, f"pb{i}", [128, 512], F32) for i in range(2)]
        py = [ps(ph, nc, f"py{i}", [128, 512], F32) for i in range(2)]
        def load_w(e, ms_):
            for m in ms_:
                v = wsrc[m][e].rearrange("(kc p) n -> p kc n", p=128)
                for hh in range(2):
                    tk.dma("pool", wbuf[e % 2][m][:, hh * 4:(hh + 1) * 4, :], v[:, hh * 4:(hh + 1) * 4, :],
                           w=[("wexp", e % 2, m, hh)], stream=f"we{e % 2}{m}{hh}")

        def gather(e):
            for j in range(4):
                tk.dma("pool", xe[(e % 2) * 4 + j][:], h2_d, r=[("idx", e)], w=[("xe", e % 2, j)],
                       stream=f"xg{e % 2}{j}",
                       indirect=dict(out_offset=None,
                                     in_offset=bass.IndirectOffsetOnAxis(ap=idx_i[:, e * 4 + j:e * 4 + j + 1], axis=0)))

        def transpose_kc(e, kc):
            for j in range(4):
                tk.op("pe", lambda pe, j=j: pe.transpose(
                    tpx[:, 0, j, :], xe[(e % 2) * 4 + j][:, kc * 128:(kc + 1) * 128], ident_b[:]),
                    r=[("xe", e % 2, j), "ident_b"], w=["tpx"], inc=(j == 3))
            if kc % 2 == 0:
                tk.op("act", lambda a: a.copy(out=xeT[e % 2][:, kc, :], in_=tpx[:, 0, :, :]),
                      r=["tpx"], w=[("xeT", e % 2, kc)])
            else:
                tk.op("dve", lambda v: v.tensor_copy(xeT[e % 2][:, kc, :], tpx[:, 0, :, :]),
                      r=["tpx"], w=[("xeT", e % 2, kc)])

        def transposes(e):
            for kc in range(8):
                transpose_kc(e, kc)

        build_idx(0)
        gather(0)
        build_idx(1)
        transposes(0)
        yc = [0]
        for e in range(NE):
            wb = wbuf[e % 2]
            wk = lambda m: [("wexp", e % 2, m, 0), ("wexp", e % 2, m, 1)]
            if e + 1 < NE:
                gather(e + 1)
            xk = [("xeT", e % 2, kc) for kc in range(8)]
            bgen = build_idx_gen(e + 2) if e + 2 < NE else iter(())

            def bstep(k):
                for _ in range(k):
                    next(bgen, None)

            for fc in range(8):
                i2 = fc % 2
                for kc in range(8):
                    tk.op("pe", lambda pe, kc=kc: pe.matmul(pa[i2][:], lhsT=wb[0][:, kc, fc * 128:(fc + 1) * 128],
                                                            rhs=xeT[e % 2][:, kc, :], start=(kc == 0), stop=(kc == 7)),
                          r=xk + wk(0), w=[("pa", i2)], inc=(kc == 7))
                for kc in range(8):
                    tk.op("pe", lambda pe, kc=kc: pe.matmul(pb[i2][:], lhsT=wb[1][:, kc, fc * 128:(fc + 1) * 128],
                                                            rhs=xeT[e % 2][:, kc, :], start=(kc == 0), stop=(kc == 7)),
                          r=xk + wk(1), w=[("pb", i2)], inc=(kc == 7))
                tk.op("act", lambda a: a.activation(out=thh[i2][:], in_=pa[i2][:], func=AF.Tanh, scale=0.5),
                      r=[("pa", i2)], w=[("thh", i2)])
                tk.op("dve", lambda v: v.scalar_tensor_tensor(out=t1h[i2][:], in0=thh[i2][:], scalar=1.0, in1=pa[i2][:],
                                                              op0=ALU.add, op1=ALU.mult),
                      r=[("thh", i2), ("pa", i2)], w=[("t1h", i2)])
                tk.op("dve", lambda v: v.scalar_tensor_tensor(out=hT[:, fc, :], in0=t1h[i2][:], scalar=0.5,
                                                              in1=pb[i2][:], op0=ALU.mult, op1=ALU.mult),
                      r=[("t1h", i2), ("pb", i2)], w=[("hT6", fc)])
                bstep(2)
            if e + 2 < NE:
                load_w(e + 2, (0, 1))
            hk = [("hT6", fc) for fc in range(8)]
            for j in range(4):
                ys_ = yc[0] % 4
                yc[0] += 1
                for hh in range(2):
                    i2 = hh
                    for fc in range(8):
                        tk.op("pe", lambda pe, fc=fc: pe.matmul(py[i2][:], lhsT=hT[:, fc, j * 128:(j + 1) * 128],
                                                                rhs=wb[2][:, fc, hh * 512:(hh + 1) * 512],
                                                                start=(fc == 0), stop=(fc == 7)),
                              r=hk + wk(2), w=[("py", i2)], inc=(fc == 7))
                    tk.op("dve", lambda v: v.scalar_tensor_tensor(
                        out=ysc[ys_][:, hh * 512:(hh + 1) * 512], in0=py[i2][:], scalar=gsel[:, e * 4 + j:e * 4 + j + 1],
                        in1=g2bc[:, hh * 512:(hh + 1) * 512], op0=ALU.mult, op1=ALU.mult),
                        r=[("py", i2), ("gsel", e), ("gbc", 1)], w=[("ysc", ys_, hh)])
                    if e + 1 < NE:
                        transpose_kc(e + 1, j * 2 + hh)
                prev = [("outacc", e - 1, jj) for jj in range(4)] if e > 0 else []
                tk.dma("pool", out_d, ysc[ys_][:], r=[("ysc", ys_, 0), ("ysc", ys_, 1), ("idx", e)] + prev,
                       w=[("outacc", e, j)], stream=f"sc{j}",
                       indirect=dict(out_offset=bass.IndirectOffsetOnAxis(ap=idx_i[:, e * 4 + j:e * 4 + j + 1], axis=0),
                                     in_offset=None, compute_op=ALU.add))
                bstep(4)
            bstep(NT + 1)
            if e + 2 < NE:
                load_w(e + 2, (2,))
        tk.barrier()
    moe.close()
    return finish(B, out_d, [])


def finish(B, out_d, extra):
    tk = B.tk
    tk.wait_all("sp")
    tk.wait_all("pool")
    B.root.close()
    return B.nc


def make_in_maps(inputs):
    consts = host_consts()
    maps = []
    shared = {
        "w_mod": np.ascontiguousarray(inputs["w_mod"][0], dtype=np.float32),
        "w_in": np.ascontiguousarray(inputs["w_in"][0], dtype=np.float32),
        "lru_w_a": np.ascontiguousarray(inputs["lru_w_a"][0], dtype=np.float32),
        "lru_w_x": np.ascontiguousarray(inputs["lru_w_x"][0], dtype=np.float32),
        "w_out": np.ascontiguousarray(inputs["w_out"][0], dtype=np.float32),
        "w1": np.ascontiguousarray(inputs["w1"][0], dtype=np.float32),
        "w3": np.ascontiguousarray(inputs["w3"][0], dtype=np.float32),
        "w2": np.ascontiguousarray(inputs["w2"][0], dtype=np.float32),
        "rel_bias": np.ascontiguousarray(np.repeat(np.asarray(inputs["rel_bias"], np.float32)[:, :, None], 128, axis=2)),
    }
    shared.update(consts)
    for b in range(8):
        m = dict(shared)
        m["x"] = np.ascontiguousarray(inputs["x"][b], dtype=np.float32)
        m["smalls"] = pack_smalls(inputs, b)
        maps.append(m)
    return maps


def kernel(**inputs):
    inputs = {k: np.asarray(v) for k, v in inputs.items()}
    nc = build()
    res = run_bass_kernel_spmd(nc, make_in_maps(inputs), core_ids=list(range(8)))
    return np.stack([np.asarray(r["out"], dtype=np.float32) for r in res.results], axis=0)
```
